# Optimizing a Trainium2 kernel written in Bass

```python
import math
import jax, jax.numpy as jnp
from jax import lax
import numpy as np


D_MODEL = 1024
BATCH = 4
SEQ = 8192
DEPTH = 1

MEM_LEN = 256
D_MIX = 2 * D_MODEL
CONV_CH = D_MIX // 2
CONV_GROUPS = 16
CONV_K = 31
SSD_INNER = D_MIX // 2
SSD_HEAD_DIM = 64
SSD_HEADS = SSD_INNER // SSD_HEAD_DIM
SSD_STATE = 128
SSD_GROUPS = 2
SSD_HEADS_PER_GROUP = SSD_HEADS // SSD_GROUPS
SSD_CONV_K = 4
SSD_CHUNK = 128
SSD_BC = SSD_GROUPS * SSD_STATE
SSD_CONV_CH = SSD_INNER + 2 * SSD_BC
IN_SPLIT_POINTS = (CONV_CH, 2 * CONV_CH, 2 * CONV_CH + SSD_INNER, 2 * CONV_CH + 2 * SSD_INNER,
                   2 * CONV_CH + 2 * SSD_INNER + SSD_BC, 2 * CONV_CH + 2 * SSD_INNER + 2 * SSD_BC)
D_IN_PROJ = 2 * CONV_CH + 2 * SSD_INNER + 2 * SSD_BC + SSD_HEADS
XA_HEADS = 4
XA_HEAD_DIM = D_MODEL // XA_HEADS
N_EXPERT_GROUPS = 4
EXPERTS_PER_GROUP = 8
N_EXPERTS = N_EXPERT_GROUPS * EXPERTS_PER_GROUP
TOP_K = 2
D_EXPERT = 512
MOE_BLOCK = 256
RMS_EPS = 1e-6
LN_EPS = 1e-5
DT_MIN = 1e-3
DT_MAX = 1e-1

kernel_name = 'hymba_conformer_ssd_xattn_hiermoe'


def rms_norm(x, g):
    x32 = x.astype(jnp.float32)
    y = x32 * lax.rsqrt(jnp.mean(x32 * x32, axis=-1, keepdims=True) + RMS_EPS)
    return (y * g.astype(jnp.float32)).astype(x.dtype)


def layer_norm(x, g, b):
    x32 = x.astype(jnp.float32)
    mu = jnp.mean(x32, axis=-1, keepdims=True)
    var = jnp.mean(jnp.square(x32 - mu), axis=-1, keepdims=True)
    y = (x32 - mu) * lax.rsqrt(var + LN_EPS)
    return (y * g.astype(jnp.float32) + b.astype(jnp.float32)).astype(x.dtype)


def causal_depthwise_conv(x, w, b):
    k = w.shape[0]
    y = lax.conv_general_dilated(x, w[:, None, :].astype(x.dtype), window_strides=(1,),
                                 padding=[(k - 1, 0)],
                                 dimension_numbers=('NWC', 'WIO', 'NWC'),
                                 feature_group_count=x.shape[-1])
    return y + b.astype(x.dtype)


def ssd_chunk_scan(x, dt, a, bm, cm, d_skip):
    bsz, seqlen = x.shape[0], x.shape[1]
    n_chunks = seqlen // SSD_CHUNK

    def to_chunks(t):
        return jnp.moveaxis(t.reshape((bsz, n_chunks, SSD_CHUNK) + t.shape[2:]), 1, 0)

    causal = jnp.tril(jnp.ones((SSD_CHUNK, SSD_CHUNK), dtype=bool))

    def step(state, inp):
        xc, dtc, bc, cc = inp
        a_cs = jnp.cumsum(dtc * a, axis=1)
        a_t = a_cs.transpose(0, 2, 1)
        seg = a_t[:, :, :, None] - a_t[:, :, None, :]
        decay_mat = jnp.exp(jnp.where(causal, seg, -jnp.inf))
        cb = jnp.repeat(jnp.einsum('btgn,bsgn->bgts', cc, bc), SSD_HEADS_PER_GROUP, axis=1)
        m = cb * decay_mat * dtc.transpose(0, 2, 1)[:, :, None, :]
        y_diag = jnp.einsum('bhts,bshp->bthp', m, xc)
        ch = jnp.repeat(cc, SSD_HEADS_PER_GROUP, axis=2)
        bh = jnp.repeat(bc, SSD_HEADS_PER_GROUP, axis=2)
        y_off = jnp.einsum('bthn,bhpn->bthp', ch, state) * jnp.exp(a_cs)[..., None]
        w_end = jnp.exp(a_cs[:, -1:, :] - a_cs) * dtc
        new_state = (state * jnp.exp(a_cs[:, -1, :])[:, :, None, None]
                     + jnp.einsum('bshn,bsh,bshp->bhpn', bh, w_end, xc))
        return new_state, y_diag + y_off + xc * d_skip[:, None]

    state0 = jnp.zeros((bsz, SSD_HEADS, SSD_HEAD_DIM, SSD_STATE), jnp.float32)
    _, ys = lax.scan(step, state0, (to_chunks(x), to_chunks(dt), to_chunks(bm), to_chunks(cm)))
    return jnp.moveaxis(ys, 0, 1).reshape(bsz, seqlen, SSD_HEADS, SSD_HEAD_DIM)


def parallel_mixer(h, w_in, conv_w, conv_b, ln_g, ln_b, ssd_conv_w, ssd_conv_b,
                   dt_bias, a_log, d_skip, ssd_norm_g, w_out):
    bsz, seqlen, _ = h.shape
    proj = h @ w_in
    c_val, c_gate, z, xs, bs, cs, dt_raw = jnp.split(proj, IN_SPLIT_POINTS, axis=-1)
    u = c_val * jax.nn.sigmoid(c_gate)
    u = causal_depthwise_conv(u, conv_w, conv_b)
    u = jax.nn.silu(layer_norm(u, ln_g, ln_b))
    xbc = jnp.concatenate([xs, bs, cs], axis=-1)
    xbc = jax.nn.silu(causal_depthwise_conv(xbc, ssd_conv_w, ssd_conv_b))
    xs, bs, cs = jnp.split(xbc, (SSD_INNER, SSD_INNER + SSD_BC), axis=-1)
    dt = jax.nn.softplus(dt_raw.astype(jnp.float32) + dt_bias.astype(jnp.float32))
    a = -jnp.exp(a_log.astype(jnp.float32))
    y = ssd_chunk_scan(xs.reshape(bsz, seqlen, SSD_HEADS, SSD_HEAD_DIM).astype(jnp.float32), dt, a,
                       bs.reshape(bsz, seqlen, SSD_GROUPS, SSD_STATE).astype(jnp.float32),
                       cs.reshape(bsz, seqlen, SSD_GROUPS, SSD_STATE).astype(jnp.float32),
                       d_skip.astype(jnp.float32))
    y = y.reshape(bsz, seqlen, SSD_INNER) * jax.nn.silu(z.astype(jnp.float32))
    yg = y.reshape(bsz, seqlen, SSD_GROUPS, SSD_INNER // SSD_GROUPS)
    yg = yg * lax.rsqrt(jnp.mean(yg * yg, axis=-1, keepdims=True) + RMS_EPS)
    y = (yg.reshape(bsz, seqlen, SSD_INNER) * ssd_norm_g.astype(jnp.float32)).astype(h.dtype)
    return jnp.concatenate([u, y], axis=-1) @ w_out


def memory_cross_attention(h, m, w_q, w_k, w_v, w_o):
    bsz, seqlen, _ = h.shape
    q = (h @ w_q).reshape(bsz, seqlen, XA_HEADS, XA_HEAD_DIM)
    k = (m @ w_k).reshape(bsz, MEM_LEN, XA_HEADS, XA_HEAD_DIM)
    v = (m @ w_v).reshape(bsz, MEM_LEN, XA_HEADS, XA_HEAD_DIM)
    s = jnp.einsum('bthd,bshd->bhts', q, k).astype(jnp.float32) * (XA_HEAD_DIM ** -0.5)
    p = jax.nn.softmax(s, axis=-1).astype(h.dtype)
    o = jnp.einsum('bhts,bshd->bthd', p, v).reshape(bsz, seqlen, D_MODEL)
    return o @ w_o


def hierarchical_moe(h, w_rg, b_rg, w_re, b_re, w_gate, w_up, w_down):
    bsz, seqlen, d = h.shape
    n_tok = bsz * seqlen
    hf = h.reshape(n_tok, d)
    p_group = jax.nn.softmax((hf @ w_rg + b_rg).astype(jnp.float32), axis=-1)
    p_top, g_sel = lax.top_k(p_group, 1)
    e_logits = (hf @ w_re + b_re).astype(jnp.float32).reshape(n_tok, N_EXPERT_GROUPS, EXPERTS_PER_GROUP)
    e_logits = jnp.take_along_axis(e_logits, g_sel[:, :, None], axis=1)[:, 0]
    w_top, i_top = lax.top_k(jax.nn.softmax(e_logits, axis=-1), TOP_K)
    w_top = w_top / jnp.sum(w_top, axis=-1, keepdims=True) * p_top
    e_flat = (g_sel * EXPERTS_PER_GROUP + i_top).reshape(-1).astype(jnp.int32)
    w_flat = w_top.reshape(-1)
    tok_flat = jnp.repeat(jnp.arange(n_tok, dtype=jnp.int32), TOP_K)
    n_assign = n_tok * TOP_K
    n_slots = n_assign + N_EXPERTS * MOE_BLOCK
    n_blocks = n_slots // MOE_BLOCK
    order = jnp.argsort(e_flat)
    sorted_e, sorted_tok, sorted_w = e_flat[order], tok_flat[order], w_flat[order]
    counts = jnp.bincount(e_flat, length=N_EXPERTS).astype(jnp.int32)
    cnt_start = jnp.cumsum(counts) - counts
    padded = ((counts + MOE_BLOCK - 1) // MOE_BLOCK) * MOE_BLOCK
    pad_end = jnp.cumsum(padded)
    pad_start = pad_end - padded
    dest = pad_start[sorted_e] + (jnp.arange(n_assign, dtype=jnp.int32) - cnt_start[sorted_e])
    buf_tok = jnp.zeros((n_slots,), jnp.int32).at[dest].set(sorted_tok)
    buf_w = jnp.zeros((n_slots,), h.dtype).at[dest].set(sorted_w.astype(h.dtype))
    block_e = jnp.clip(jnp.searchsorted(pad_end, jnp.arange(n_blocks, dtype=jnp.int32) * MOE_BLOCK,
                                        side='right'), 0, N_EXPERTS - 1).astype(jnp.int32)
    x_blocks = hf[buf_tok].reshape(n_blocks, MOE_BLOCK, d)

    def expert_block(args):
        xb, e = args
        return (jax.nn.silu(xb @ w_gate[e]) * (xb @ w_up[e])) @ w_down[e]

    y_blocks = lax.map(expert_block, (x_blocks, block_e)).reshape(n_slots, d)
    out = jnp.zeros((n_tok, d), h.dtype).at[buf_tok].add(y_blocks * buf_w[:, None])
    return out.reshape(bsz, seqlen, d)


def setup_inputs(seed: int = 0) -> dict:
    key = jax.random.key(seed)
    ks = jax.random.split(key, 32)
    f32 = jnp.float32

    def nrm(k, shape, scale):
        return jax.random.normal(k, shape, f32) * scale

    def gain(k, shape):
        return 1.0 + 0.02 * jax.random.normal(k, shape, f32)

    dt0 = jnp.exp(jax.random.uniform(ks[10], (DEPTH, SSD_HEADS), f32,
                                     math.log(DT_MIN), math.log(DT_MAX)))
    dt_bias = dt0 + jnp.log(-jnp.expm1(-dt0))
    return {
        'x': jax.random.normal(ks[0], (BATCH, SEQ, D_MODEL), f32),
        'mem': jax.random.normal(ks[1], (BATCH, MEM_LEN, D_MODEL), f32),
        'g_mix': gain(ks[2], (DEPTH, D_MODEL)),
        'w_in': nrm(ks[3], (DEPTH, D_MODEL, D_IN_PROJ), D_MODEL ** -0.5),
        'conv_w': nrm(ks[4], (DEPTH, CONV_K, CONV_CH), CONV_K ** -0.5),
        'conv_b': nrm(ks[5], (DEPTH, CONV_CH), 0.02),
        'ln_g': gain(ks[6], (DEPTH, CONV_CH)),
        'ln_b': nrm(ks[7], (DEPTH, CONV_CH), 0.02),
        'ssd_conv_w': nrm(ks[8], (DEPTH, SSD_CONV_K, SSD_CONV_CH), SSD_CONV_K ** -0.5),
        'ssd_conv_b': nrm(ks[9], (DEPTH, SSD_CONV_CH), 0.02),
        'dt_bias': dt_bias,
        'a_log': jnp.log(jax.random.uniform(ks[11], (DEPTH, SSD_HEADS), f32, 1.0, 16.0)),
        'd_skip': gain(ks[12], (DEPTH, SSD_HEADS)),
        'ssd_norm_g': gain(ks[13], (DEPTH, SSD_INNER)),
        'w_out': nrm(ks[14], (DEPTH, D_MIX, D_MODEL), D_MIX ** -0.5),
        'g_xattn': gain(ks[15], (DEPTH, D_MODEL)),
        'g_mem': gain(ks[16], (DEPTH, D_MODEL)),
        'w_q': nrm(ks[17], (DEPTH, D_MODEL, D_MODEL), D_MODEL ** -0.5),
        'w_k': nrm(ks[18], (DEPTH, D_MODEL, D_MODEL), D_MODEL ** -0.5),
        'w_v': nrm(ks[19], (DEPTH, D_MODEL, D_MODEL), D_MODEL ** -0.5),
        'w_o': nrm(ks[20], (DEPTH, D_MODEL, D_MODEL), D_MODEL ** -0.5),
        'g_moe': gain(ks[21], (DEPTH, D_MODEL)),
        'w_router_group': nrm(ks[22], (DEPTH, D_MODEL, N_EXPERT_GROUPS), D_MODEL ** -0.5),
        'b_router_group': nrm(ks[23], (DEPTH, N_EXPERT_GROUPS), 0.01),
        'w_router_expert': nrm(ks[24], (DEPTH, D_MODEL, N_EXPERTS), D_MODEL ** -0.5),
        'b_router_expert': nrm(ks[25], (DEPTH, N_EXPERTS), 0.01),
        'w_gate': nrm(ks[26], (DEPTH, N_EXPERTS, D_MODEL, D_EXPERT), D_MODEL ** -0.5),
        'w_up': nrm(ks[27], (DEPTH, N_EXPERTS, D_MODEL, D_EXPERT), D_MODEL ** -0.5),
        'w_down': nrm(ks[28], (DEPTH, N_EXPERTS, D_EXPERT, D_MODEL), D_EXPERT ** -0.5),
        'g_final': gain(ks[29], (D_MODEL,)),
    }


def reference(x, mem, g_mix, w_in, conv_w, conv_b, ln_g, ln_b, ssd_conv_w, ssd_conv_b,
              dt_bias, a_log, d_skip, ssd_norm_g, w_out, g_xattn, g_mem, w_q, w_k, w_v, w_o,
              g_moe, w_router_group, b_router_group, w_router_expert, b_router_expert,
              w_gate, w_up, w_down, g_final):
    for l in range(DEPTH):
        h = rms_norm(x, g_mix[l])
        x = x + parallel_mixer(h, w_in[l], conv_w[l], conv_b[l], ln_g[l], ln_b[l],
                               ssd_conv_w[l], ssd_conv_b[l], dt_bias[l], a_log[l],
                               d_skip[l], ssd_norm_g[l], w_out[l])
        h = rms_norm(x, g_xattn[l])
        m = rms_norm(mem, g_mem[l])
        x = x + memory_cross_attention(h, m, w_q[l], w_k[l], w_v[l], w_o[l])
        h = rms_norm(x, g_moe[l])
        x = x + hierarchical_moe(h, w_router_group[l], b_router_group[l], w_router_expert[l],
                                 b_router_expert[l], w_gate[l], w_up[l], w_down[l])
    return rms_norm(x, g_final)
```

```python
import contextlib
import numpy as np
import concourse.bass as bass
import concourse.mybir as mybir
from concourse.bass_utils import run_bass_kernel_spmd

F32 = mybir.dt.float32
BF16 = mybir.dt.bfloat16
I32 = mybir.dt.int32
AF = mybir.ActivationFunctionType
ALU = mybir.AluOpType
AX = mybir.AxisListType

ENGS = ("pe", "act", "dve", "pool", "sp")
SEM_CHUNK = 30000


class Op:
    __slots__ = ("eng", "fn", "deps", "dma", "sig", "idx", "dman", "seq")

    def __init__(self, eng, fn, dma):
        self.eng = eng
        self.fn = fn
        self.deps = set()
        self.dma = dma
        self.sig = False
        self.idx = -1
        self.dman = 0
        self.seq = 0


class Sched:
    def __init__(self, nc):
        self.nc = nc
        self.ops = {e: [] for e in ENGS}
        self.lastw = {}
        self.readers = {}
        self.subs = {}
        self.dma_cnt = {}
        self.nops = 0
        self.limit = None
        self.bar = set()
        self.dma_last = {}
        self.marks = []
        self.skipped = 0

    def mark(self, name):
        self.marks.append((name, self.nops))

    def _expand(self, key, record):
        name, sub = key if isinstance(key, tuple) else (key, None)
        s = self.subs.setdefault(name, set())
        if sub is None:
            return [(name, None)] + [(name, x) for x in s]
        s.add(sub)
        if record:
            return [(name, sub)]
        return [(name, sub), (name, None)]

    def add(self, eng, fn, reads=(), writes=(), dma=None, force=False):
        if self.limit is not None and self.nops >= self.limit and not force:
            self.skipped += 1
            return None
        op = Op(eng, fn, dma)
        op.seq = self.nops
        self.nops += 1
        for k in reads:
            for kk in self._expand(k, False):
                w = self.lastw.get(kk)
                if w is not None:
                    op.deps.add(w)
        for k in writes:
            for kk in self._expand(k, False):
                w = self.lastw.get(kk)
                if w is not None:
                    op.deps.add(w)
                for r in self.readers.get(kk, ()):
                    op.deps.add(r)
        op.deps |= self.bar
        op.deps.discard(op)
        for k in reads:
            for kk in self._expand(k, True):
                lst = self.readers.setdefault(kk, [])
                if dma is None:
                    lst[:] = [r for r in lst if not (r.dma is None and r.eng == eng)]
                lst.append(op)
        for k in writes:
            for kk in self._expand(k, True):
                self.lastw[kk] = op
                self.readers[kk] = []
        if dma is not None:
            n = self.dma_cnt.get(dma, 0) + 1
            self.dma_cnt[dma] = n
            op.dman = n
            self.dma_last[dma] = op
        self.ops[eng].append(op)
        return op

    def barrier(self):
        deps = set(self.dma_last.values())
        for e in ENGS:
            for op in reversed(self.ops[e]):
                if op.dma is None:
                    deps.add(op)
                    op.sig = True
                    break
        self.bar = deps

    def emit_phase(self, final=False):
        nc = self.nc
        if not hasattr(self, "_st"):
            self._st = contextlib.ExitStack()
            self._esems = {e: [self._st.enter_context(nc.semaphore(f"s_{e}{i}")) for i in range(3)] for e in ENGS}
            self._dsems = {}
            self._start = {e: 0 for e in ENGS}
            self._cnt = {e: 0 for e in ENGS}
            self._waited = {e: {} for e in ENGS}
            self._emitted = set()
            self._prevbar = set()
        esems, dsems = self._esems, self._dsems
        cur = {e: self.ops[e][self._start[e]:] for e in ENGS}
        curset = set()
        for e in ENGS:
            curset.update(cur[e])
        for e in ENGS:
            for op in cur[e]:
                best = {}
                keep = set()
                for d in op.deps:
                    if d.dma is not None:
                        keep.add(d)
                        continue
                    if d not in curset and d not in self._prevbar:
                        continue
                    if d.eng == "pe" and op.eng == "pe" and op.dma is None:
                        continue
                    b = best.get(d.eng)
                    if b is None or d.seq > b.seq:
                        best[d.eng] = d
                keep.update(best.values())
                op.deps = keep
                for d in keep:
                    if d.dma is None:
                        assert d in curset or d.sig
                        d.sig = True
        for e in ENGS:
            for op in cur[e]:
                if op.dma is None and op.sig:
                    op.idx = self._cnt[e]
                    self._cnt[e] += 1
        for k in self.dma_cnt:
            if k not in dsems:
                dsems[k] = self._st.enter_context(nc.semaphore(f"d_{len(dsems)}"))
        with nc.Block() as block:
            def run(engname, eng):
                waited = self._waited[engname]
                for op in cur[engname]:
                    need = {}
                    for d in sorted(op.deps, key=lambda o: o.seq):
                        if d.dma is not None:
                            sem, val = dsems[d.dma], 16 * d.dman
                        else:
                            sem, val = esems[d.eng][d.idx // SEM_CHUNK], d.idx % SEM_CHUNK + 1
                        kk = id(sem)
                        if need.get(kk, (None, 0))[1] < val:
                            need[kk] = (sem, val)
                    for kk, (sem, val) in need.items():
                        if waited.get(kk, 0) >= val:
                            continue
                        waited[kk] = val
                        eng.wait_ge(sem, val)
                    inst = op.fn(eng)
                    if op.dma is not None:
                        inst.then_inc(dsems[op.dma], 16)
                    elif op.sig:
                        inst.then_inc(esems[engname][op.idx // SEM_CHUNK], 1)
                if engname == "sp" and final:
                    for k, n in self.dma_cnt.items():
                        eng.wait_ge(dsems[k], 16 * n)

            @block.tensor
            def _(eng):
                run("pe", eng)

            @block.scalar
            def _(eng):
                run("act", eng)

            @block.vector
            def _(eng):
                run("dve", eng)

            @block.gpsimd
            def _(eng):
                run("pool", eng)

            @block.sync
            def _(eng):
                run("sp", eng)
        for e in ENGS:
            self._start[e] = len(self.ops[e])
        self._prevbar = set(self.bar)
        if final:
            self._st.close()


D = 1024
DIN = 4624
T = 256
NE = 32
BLK = 256
RMS_EPS = 1e-6
LN_EPS = 1e-5
BIGS = 65536.0
NPE = 12
FDRIP = 2
DRIP = 1

CV = {}
_off = 0
for _n, _w in [("g_mix", 8), ("cw", 8 * 31), ("cb", 8), ("ln_g", 8), ("ln_b", 8), ("w4", 48), ("b4", 12),
               ("dt_bias", 1), ("a_log", 16), ("dsk", 16), ("ng", 8), ("g_xa", 8), ("g_mem", 8), ("g_moe", 8),
               ("rb", 36), ("flag", 1), ("thr", 16), ("iob", 64), ("pid2", 1)]:
    CV[_n] = (_off, _w)
    _off += _w
NCV = _off


def pk(v, k):
    return np.ascontiguousarray(np.asarray(v, np.float32).reshape(k, 128).T)


def build(nt, dbg=None, limit=None):
    NTOK = nt * T
    NCHK = NTOK // 128
    NSLOT_BLKS = (2 * NTOK + NE * (BLK - 1)) // BLK
    NSLOTS = NSLOT_BLKS * BLK
    nc = bass.Bass("TRN2", target_bir_lowering=False)
    dt_in = lambda name, shape, dt=F32: nc.dram_tensor(name, shape, dt, kind="ExternalInput").ap()
    xT_d = dt_in("xT", [D, 2 * NTOK])
    memT_d = dt_in("memT", [D, 256])
    cv_d = dt_in("cv", [128, NCV])
    cm_d = dt_in("cm", [128, 640])
    gbc_d = dt_in("gbc", [128, 2 * D])
    w_in_d = dt_in("w_in", [D, DIN])
    w_out_d = dt_in("w_out", [2 * D, D])
    w_q_d = dt_in("w_q", [D, D])
    w_k_d = dt_in("w_k", [D, D])
    w_v_d = dt_in("w_v", [D, D])
    w_o_d = dt_in("w_o", [D, D])
    w_r_d = dt_in("w_r", [D, 36])
    w_gate_d = dt_in("w_gate_r", [NE * 256, 2048])
    w_up_d = dt_in("w_up_r", [NE * 256, 2048])
    w_down_d = dt_in("w_down_r", [NE * 256, 2048])
    out_d = nc.dram_tensor("out", [NTOK, D], F32, kind="ExternalOutput").ap()
    CAT_d = nc.dram_tensor("CAT", [2 * D, NTOK], BF16, kind="Internal").ap()
    X2_d = nc.dram_tensor("X2", [NTOK, D], F32, kind="Internal").ap()
    H3_d = nc.dram_tensor("H3", [NTOK, D], BF16, kind="Internal").ap()
    Xs_d = nc.dram_tensor("Xs", [NSLOTS, D], BF16, kind="Internal").ap()
    Ys_d = nc.dram_tensor("Ys", [NSLOTS, D], F32, kind="Internal").ap()
    dbg_d = {}
    if dbg:
        for name, shape in dbg.items():
            dbg_d[name] = nc.dram_tensor("dbg_" + name, shape, F32, kind="ExternalOutput").ap()

    S = Sched(nc)
    S.limit = limit
    xT_v = xT_d.rearrange("(ko p) t -> p ko t", p=128)
    CAT_v = CAT_d.rearrange("(ko p) t -> p ko t", p=128)

    def bc3(ap2, n):
        return ap2.unsqueeze(2).to_broadcast((ap2.shape[0], ap2.shape[1], n))

    def bcm(ap2, n):
        return ap2.unsqueeze(1).to_broadcast((ap2.shape[0], n, ap2.shape[1]))

    with contextlib.ExitStack() as top:
        def sb(name, shape, dt, st=top):
            return st.enter_context(nc.sbuf_tensor(name + "_sb", shape, dt))

        def ps(name, shape, dt, st=top):
            return st.enter_context(nc.psum_tensor(name + "_ps", shape, dt))

        cv = sb("cv", [128, NCV], F32)
        cm = sb("cm", [128, 640], F32)
        cmb = sb("cmb", [128, 384], BF16)
        Abc = sb("Abc", [128, 16], F32)
        S.add("sp", lambda e: e.dma_start(out=cv[:], in_=cv_d), writes=["cv"], dma="c0")
        S.add("sp", lambda e: e.dma_start(out=cm[:], in_=cm_d), writes=["cm"], dma="c1")
        identf = cm[:, 0:128]
        onesf = cm[:, 128:256]
        tri_incl = cm[:, 256:384]
        stri_gt = cm[:, 384:512]
        S.add("dve", lambda e: e.tensor_copy(out=cmb[:, 0:256], in_=cm[:, 0:256]), reads=["cm"], writes=[("cmb", 0)])
        S.add("dve", lambda e: e.tensor_copy(out=cmb[:, 256:384], in_=cm[:, 512:640]), reads=["cm"], writes=[("cmb", 1)])
        ident = cmb[:, 0:128]
        ones = cmb[:, 128:256]
        stri_lt = cmb[:, 256:384]
        S.add("act", lambda e: e.activation(out=Abc[:], in_=cv[:, CV["a_log"][0]:CV["a_log"][0] + 16], func=AF.Exp), reads=["cv"], writes=["Abc"])
        S.add("dve", lambda e: e.tensor_scalar(out=Abc[:], in0=Abc[:], scalar1=-1.0, scalar2=None, op0=ALU.mult), reads=["Abc"], writes=["Abc"])

        def cvs(name, j=0, n=1):
            o = CV[name][0] + j
            return cv[:, o:o + n]

        epsr = sb("epsr", [128, 4], F32)
        S.add("dve", lambda e: e.memset(epsr[:, 0:1], RMS_EPS), writes=[("eps", 0)])
        S.add("dve", lambda e: e.memset(epsr[:, 1:2], LN_EPS), writes=[("eps", 1)])
        S.add("dve", lambda e: e.memset(epsr[:, 2:3], 1.0), writes=[("eps", 2)])
        ereg_top = top.enter_context(nc.gpsimd.register("ereg"))

        with contextlib.ExitStack() as pa:
            A_sb = lambda n, s, d: sb(n, s, d, pa)
            w_inT = A_sb("w_inT", [128, 8, DIN], BF16)
            win_v = w_in_d.rearrange("(ko p) n -> p ko n", p=128)
            for k in range(8):
                for (a, b) in [(0, 2048), (2048, 4096), (4096, DIN)]:
                    S.add("pool", lambda e, k=k, a=a, b=b: e.dma_start(out=w_inT[:, k, a:b], in_=win_v[:, k, a:b]),
                          writes=[("w_inT", (k, a))], dma="w_in")

            dg = A_sb("dg", [128, NPE * 8, 128], BF16)
            for k in range(NPE):
                for c in range(8):
                    S.add("dve", lambda e, k=k, c=c: e.tensor_scalar(out=dg[:, k * 8 + c, :], in0=ident, scalar1=cvs("cw", c * 31 + k), scalar2=None, op0=ALU.mult),
                          reads=["cmb", "cv"], writes=[("dg", k * 8 + c)])
            xt = A_sb("xt", [128, 8, T], F32)
            hT = A_sb("hT", [128, 8, T], BF16)
            sq = hT
            stt_ = [A_sb(f"stt{i}", [128, T], F32) for i in range(4)]
            rstd, mean_b, tmpa, tmpb = stt_
            sg = [A_sb(f"sg{i}", [128, T], BF16) for i in range(2)]
            upres = [A_sb(f"upre{i}", [128, 8, 30 + T], BF16) for i in range(2)]
            acc = A_sb("acc", [128, 8, T], F32)
            cat = A_sb("cat", [128, 16, T], BF16)
            szTs = [A_sb(f"szT{i}", [128, 8, T], BF16) for i in range(2)]
            xbp = A_sb("xbp", [128, 12, 3 + T], BF16)
            xbcTs = [A_sb(f"xbcT{i}", [128, 12, T], BF16) for i in range(2)]
            dgt = A_sb("dgt", [128, 8, 128], BF16)
            dcnt = [0]
            dtTs = [A_sb(f"dtT{i}", [16, T], F32) for i in range(2)]
            ygT = A_sb("ygT", [128, 8, T], BF16)
            tmp16 = A_sb("tmp16", [16, T], F32)
            xtok = A_sb("xtok", [128, 1024], BF16)
            xdt = A_sb("xdt", [128, 1024], BF16)
            xD = A_sb("xD", [128, 1024], BF16)
            Btok = A_sb("Btok", [128, 256], BF16)
            Et = A_sb("Et", [128, 8, 128], BF16)
            CBm = A_sb("CBm", [128, 2, 128], F32)
            mT = A_sb("mT", [128, 16, 128], BF16)
            sm = A_sb("sm", [128, 8, 16], F32)
            dttok, dtA, cs_sb, eacs, dif, wend, etot = [sm[:, i, :] for i in range(7)]
            ytok = A_sb("ytok", [128, 1024], BF16)
            t1 = A_sb("t1", [128, 1024], F32)
            state = A_sb("state", [128, 1024], F32)
            stateT = A_sb("stateT", [128, 1024], BF16)
            mm = [ps(f"mm{i}", [128, 512], F32, pa) for i in range(2)]
            stp = ps("stp", [128, 512], F32, pa)
            tp = ps("tp", [128, 1024], BF16, pa)
            yps = ps("yps", [128, 1024], F32, pa)
            big = ps("big", [128, 1024], F32, pa)
            xw = xD

            S.add("dve", lambda e: e.memset(state[:], 0.0), writes=["state"])
            S.add("pool", lambda e: e.memset(stateT[:], 0.0), writes=["stateT"])
            for u_ in range(2):
                S.add("pool", lambda e, u_=u_: e.memset(upres[u_][:], 0.0), writes=[f"upre{u_}"])
            S.add("pool", lambda e: e.memset(xbp[:], 0.0), writes=["xbp"])
            nmm = [0]

            def rms_h(src, gname, dst, n=T, skey='xt', dkey='hT'):
                S.add("act", lambda e: e.activation(out=sq[:, :, 0:n], in_=src[:, :, 0:n], func=AF.Square), reads=[skey], writes=["hT"])
                pb = mm[nmm[0] % 2]
                key = f"mm{nmm[0] % 2}"
                nmm[0] += 1
                for k in range(8):
                    S.add("pe", lambda e, k=k: e.matmul(pb[:, 0:n], lhsT=ones, rhs=sq[:, k, 0:n], start=(k == 0), stop=(k == 7)),
                          reads=["hT", "cmb"], writes=[key])
                S.add("act", lambda e: e.activation(out=tmpa[:, 0:n], in_=pb[:, 0:n], func=AF.Ln, scale=1.0 / D, bias=epsr[:, 0:1]), reads=[key, "eps"], writes=["tmpa"])
                S.add("act", lambda e: e.activation(out=rstd[:, 0:n], in_=tmpa[:, 0:n], func=AF.Exp, scale=-0.5), reads=["tmpa"], writes=["rstd"])
                for k in range(8):
                    S.add("dve", lambda e, k=k: e.scalar_tensor_tensor(out=dst[:, k, 0:n], in0=src[:, k, 0:n], scalar=cvs(gname, k), in1=rstd[:, 0:n],
                                                                         op0=ALU.mult, op1=ALU.mult),
                          reads=[skey, "cv", "rstd"], writes=[(dkey, k)])

            def diag_otf(wname, widx):
                slot = dcnt[0] % 8
                dcnt[0] += 1
                S.add("act", lambda e, slot=slot: e.activation(out=dgt[:, slot, :], in_=ident, func=AF.Copy, scale=cvs(wname, widx)), reads=["cmb", "cv"], writes=[("dgt", slot)])
                return slot

            def inproj(j, ncols=128):
                pb = mm[nmm[0] % 2]
                key = f"mm{nmm[0] % 2}"
                nmm[0] += 1
                for k in range(8):
                    S.add("pe", lambda e, k=k, pb=pb: e.matmul(pb[0:ncols, 0:T], lhsT=w_inT[:, k, j * 128:j * 128 + ncols], rhs=hT[:, k, :],
                                                               start=(k == 0), stop=(k == 7)),
                          reads=["w_inT", "hT"], writes=[key])
                return pb, key

            def modeof(i):
                return "main" if i >= nt else ("pre_last" if i == nt - 1 else "pre")

            def frontA(i):
                mode = modeof(i)
                main = mode == "main"
                glu = mode in ("main", "pre_last")
                par = i % 2
                xbcT = xbcTs[par]
                xk = f"xbcT{par}"
                dtT = dtTs[par]
                dk = f"dtT{par}"
                upre, uk = upres[par], f"upre{par}"
                upo, uko = upres[1 - par], f"upre{1 - par}"
                szT, zk = szTs[par], f"szT{par}"
                S.add("sp", lambda e: e.dma_start(out=xt[:], in_=xT_v[:, :, i * T:(i + 1) * T]), writes=["xt"], dma="xt")
                rms_h(xt, "g_mix", hT)
                yield
                nxc = 12 if glu else 10
                for c in range(nxc):
                    pb, key = inproj(24 + c)
                    S.add("act", lambda e, pb=pb, c=c: e.activation(out=xbp[:, c, 3:3 + T], in_=pb[:, 0:T], func=AF.Copy), reads=[key], writes=[("xbp", c)])
                    pb2 = mm[nmm[0] % 2]
                    key2 = f"mm{nmm[0] % 2}"
                    nmm[0] += 1
                    for k in range(4):
                        slot = diag_otf("w4", c * 4 + k)
                        S.add("pe", lambda e, c=c, k=k, slot=slot, pb2=pb2: e.matmul(pb2[:, 0:T], lhsT=dgt[:, slot, :], rhs=xbp[:, c, k:k + T], start=(k == 0), stop=(k == 3)),
                              reads=[("xbp", c), ("dgt", slot)], writes=[key2])
                    S.add("act", lambda e, c=c, pb2=pb2: e.activation(out=xbcT[:, c, :], in_=pb2[:, 0:T], func=AF.Silu, bias=cvs("b4", c)), reads=[key2, "cv"], writes=[(xk, c)])
                    yield
                S.add("pool", lambda e: e.tensor_copy(out=xbp[:, :, 0:3], in_=xbp[:, :, T:T + 3]), reads=["xbp"], writes=["xbp"])
                pb, key = inproj(36, 16)
                S.add("act", lambda e, pb=pb: e.activation(out=tmp16[:], in_=pb[0:16, 0:T], func=AF.Exp, bias=cv[0:16, CV["dt_bias"][0]:CV["dt_bias"][0] + 1]),
                      reads=[key, "cv"], writes=["tmp16"])
                S.add("act", lambda e: e.activation(out=dtT[:], in_=tmp16[:], func=AF.Ln, bias=epsr[0:16, 2:3]), reads=["tmp16", "eps"], writes=[dk])
                yield
                if main:
                    for c in range(8):
                        pb, key = inproj(16 + c)
                        S.add("act", lambda e, pb=pb, c=c: e.activation(out=szT[:, c, :], in_=pb[:, 0:T], func=AF.Silu), reads=[key], writes=[(zk, c)])
                        yield
                if glu:
                    S.add("pool", lambda e: e.tensor_copy(out=upre[:, :, 0:30], in_=upo[:, :, T:T + 30]), reads=[uko], writes=[uk])
                    for c in range(8):
                        pb, key = inproj(8 + c)
                        s_ = sg[c % 2]
                        S.add("act", lambda e, pb=pb, s_=s_: e.activation(out=s_[:], in_=pb[:, 0:T], func=AF.Sigmoid), reads=[key], writes=[f"sg{c % 2}"])
                        pb, key = inproj(c)
                        S.add("dve", lambda e, pb=pb, s_=s_, c=c: e.tensor_tensor(out=upre[:, c, 30:30 + T], in0=pb[:, 0:T], in1=s_[:], op=ALU.mult),
                              reads=[key, f"sg{c % 2}"], writes=[(uk, c)])
                        yield

            def backA(i, fgen):
                mode = modeof(i)
                main = mode == "main"
                par = i % 2
                xbcT = xbcTs[par]
                xk = f"xbcT{par}"
                dtT = dtTs[par]
                dk = f"dtT{par}"
                upre, uk = upres[par], f"upre{par}"
                szT, zk = szTs[par], f"szT{par}"

                def conv31_gen():
                    for c in range(8):
                        pb = mm[nmm[0] % 2]
                        key = f"mm{nmm[0] % 2}"
                        nmm[0] += 1
                        for k in range(31):
                            if k < NPE:
                                S.add("pe", lambda e, c=c, k=k, pb=pb: e.matmul(pb[:, 0:T], lhsT=dg[:, k * 8 + c, :], rhs=upre[:, c, k:k + T], start=(k == 0), stop=(k == 30)),
                                      reads=[(uk, c), "dg"], writes=[key])
                            else:
                                slot = diag_otf("cw", c * 31 + k)
                                S.add("pe", lambda e, c=c, k=k, pb=pb, slot=slot: e.matmul(pb[:, 0:T], lhsT=dgt[:, slot, :], rhs=upre[:, c, k:k + T], start=(k == 0), stop=(k == 30)),
                                      reads=[(uk, c), ("dgt", slot)], writes=[key])
                        S.add("dve", lambda e, c=c, pb=pb: e.tensor_scalar(out=acc[:, c, :], in0=pb[:, 0:T], scalar1=cvs("cb", c), scalar2=None, op0=ALU.add),
                              reads=[key, "cv"], writes=[("acc", c)])
                        yield

                gen = conv31_gen() if main else iter(())

                def drip(n):
                    for _ in range(n):
                        if next(gen, "end") == "end":
                            break

                def fdrip(n):
                    for _ in range(n):
                        if next(fgen, "end") == "end":
                            break

                def dve(fn, reads, writes, n=DRIP):
                    drip(n)
                    if n:
                        fdrip(FDRIP)
                    S.add("dve", fn, reads=reads, writes=writes)

                def chunk(q):
                    cols = slice(q * 128, (q + 1) * 128)
                    for c in range(8):
                        S.add("pe", lambda e, c=c: e.transpose(out=tp[:, c * 128:(c + 1) * 128], in_=xbcT[:, c, cols], identity=ident), reads=[(xk, c), "cmb"], writes=["tp"])
                    S.add("act", lambda e: e.activation(out=xtok[:], in_=tp[:], func=AF.Copy), reads=["tp"], writes=["xtok"])
                    for g in range(2):
                        S.add("pe", lambda e, g=g: e.transpose(out=tp[:, g * 128:(g + 1) * 128], in_=xbcT[:, 8 + g, cols], identity=ident), reads=[(xk, 8 + g), "cmb"], writes=["tp"])
                    S.add("act", lambda e: e.activation(out=Btok[:], in_=tp[:, 0:256], func=AF.Copy), reads=["tp"], writes=["Btok"])
                    S.add("pe", lambda e: e.transpose(out=stp[:, 32:48], in_=dtT[0:16, cols], identity=identf[0:16, 0:16]), reads=[dk, "cm"], writes=["stp"])
                    dve(lambda e: e.tensor_copy(out=dttok, in_=stp[:, 32:48]), ["stp"], [("sm", 0)])
                    dve(lambda e: e.tensor_tensor(out=dtA, in0=dttok, in1=Abc[:], op=ALU.mult), [("sm", 0), "Abc"], [("sm", 1)], 0)
                    S.add("pe", lambda e: e.matmul(stp[:, 0:16], lhsT=tri_incl, rhs=dtA, start=True, stop=True), reads=[("sm", 1), "cm"], writes=["stp"])
                    S.add("pe", lambda e: e.matmul(stp[:, 16:32], lhsT=onesf, rhs=dtA, start=True, stop=True), reads=[("sm", 1), "cm"], writes=["stp"])
                    dve(lambda e: e.tensor_copy(out=cs_sb, in_=stp[:, 0:16]), ["stp"], [("sm", 2)])
                    S.add("act", lambda e: e.activation(out=eacs, in_=cs_sb, func=AF.Exp), reads=[("sm", 2)], writes=[("sm", 3)])
                    dve(lambda e: e.tensor_tensor(out=dif, in0=stp[:, 16:32], in1=cs_sb, op=ALU.subtract), ["stp", ("sm", 2)], [("sm", 4)], 0)
                    S.add("act", lambda e: e.activation(out=wend, in_=dif, func=AF.Exp), reads=[("sm", 4)], writes=[("sm", 5)])
                    S.add("act", lambda e: e.activation(out=etot, in_=stp[:, 16:32], func=AF.Exp), reads=["stp"], writes=[("sm", 6)])
                    dve(lambda e: e.tensor_tensor(out=wend, in0=wend, in1=dttok, op=ALU.mult), [("sm", 5), ("sm", 0)], [("sm", 5)])
                    xtok3 = xtok[:].rearrange("p (h d) -> p h d", d=64)
                    Rt = t1[:].rearrange("p (a b) -> p a b", b=128)
                    if main:
                        for g in range(2):
                            S.add("pe", lambda e, g=g: e.matmul(stp[:, 64 + g * 128:64 + (g + 1) * 128], lhsT=xbcT[:, 8 + g, cols], rhs=xbcT[:, 10 + g, cols], start=True, stop=True),
                                  reads=[(xk, 8 + g), (xk, 10 + g)], writes=["stp"])
                            dve(lambda e, g=g: e.tensor_tensor(out=CBm[:, g, :], in0=stp[:, 64 + g * 128:64 + (g + 1) * 128], in1=tri_incl, op=ALU.mult), ["stp", "cm"], [("CBm", g)])
                        S.add("pool", lambda e: e.tensor_tensor(out=xdt[:].rearrange("p (h d) -> p h d", d=64), in0=xtok3, in1=bc3(dttok, 64), op=ALU.mult),
                              reads=["xtok", ("sm", 0)], writes=["xdt"])
                        for half in range(2):
                            S.add("pool", lambda e, half=half: e.tensor_tensor(out=Rt, in0=bcm(tri_incl, 8), in1=bc3(dtA[:, half * 8:(half + 1) * 8], 128), op=ALU.mult),
                                  reads=["cm", ("sm", 1)], writes=["t1"])
                            for jj in range(2):
                                S.add("pe", lambda e, jj=jj: e.matmul(big[:, jj * 512:(jj + 1) * 512], lhsT=stri_gt, rhs=t1[:, jj * 512:(jj + 1) * 512],
                                                                      start=True, stop=True), reads=["t1", "cm"], writes=["big"])
                            S.add("act", lambda e: e.activation(out=Et[:].rearrange("p a b -> p (a b)"), in_=big[:], func=AF.Exp), reads=["big"], writes=["Et"])
                            dve(lambda e, half=half: e.tensor_tensor(out=mT[:, half * 8:(half + 1) * 8, :], in0=Et[:], in1=bcm(CBm[:, half, :], 8), op=ALU.mult),
                                ["Et", ("CBm", half)], [("mT", half)])
                        S.add("pool", lambda e: e.tensor_tensor(out=xD[:].rearrange("p (h d) -> p h d", d=64), in0=xtok3, in1=bc3(cvs("dsk", 0, 16), 64), op=ALU.mult),
                              reads=["xtok", "cv"], writes=["xD"])
                        for jj in range(2):
                            S.add("pe", lambda e, jj=jj: e.matmul(yps[:, jj * 512:(jj + 1) * 512], lhsT=ident, rhs=xD[:, jj * 512:(jj + 1) * 512], start=True, stop=False, skip_group_check=True),
                                  reads=["xD", "cmb"], writes=["yps"])
                        for h in range(16):
                            S.add("pe", lambda e, h=h: e.matmul(yps[:, h * 64:(h + 1) * 64], lhsT=mT[:, h, :], rhs=xdt[:, h * 64:(h + 1) * 64], start=False, stop=True, skip_group_check=True),
                                  reads=[("mT", h // 8), "xdt"], writes=["yps"])
                        for g in range(2):
                            S.add("pe", lambda e, g=g: e.matmul(big[:, g * 512:(g + 1) * 512], lhsT=xbcT[:, 10 + g, cols], rhs=stateT[:, g * 512:(g + 1) * 512], start=True, stop=True),
                                  reads=[(xk, 10 + g), "stateT"], writes=["big"])
                        dve(lambda e: e.tensor_tensor(out=t1[:].rearrange("p (h d) -> p h d", d=64), in0=big[:].rearrange("p (h d) -> p h d", d=64), in1=bc3(eacs, 64), op=ALU.mult),
                            ["big", ("sm", 3)], ["t1"])
                        dve(lambda e: e.tensor_tensor(out=ytok[:], in0=yps[:], in1=t1[:], op=ALU.add), ["yps", "t1"], ["ytok"])
                    S.add("pool", lambda e: e.tensor_tensor(out=xw[:].rearrange("p (h d) -> p h d", d=64), in0=xtok3, in1=bc3(wend, 64), op=ALU.mult), reads=["xtok", ("sm", 5)], writes=["xD"])
                    for g in range(2):
                        S.add("pe", lambda e, g=g: e.matmul(big[:, g * 512:(g + 1) * 512], lhsT=Btok[:, g * 128:(g + 1) * 128], rhs=xw[:, g * 512:(g + 1) * 512], start=True, stop=True),
                              reads=["Btok", "xD"], writes=["big"])
                    dve(lambda e: e.tensor_tensor(out=state[:].rearrange("p (h d) -> p h d", d=64), in0=state[:].rearrange("p (h d) -> p h d", d=64), in1=bc3(etot, 64), op=ALU.mult),
                        ["state", ("sm", 6)], ["state"])
                    dve(lambda e: e.tensor_tensor(out=state[:], in0=state[:], in1=big[:], op=ALU.add), ["state", "big"], ["state"])
                    S.add("act", lambda e: e.activation(out=stateT[:], in_=state[:], func=AF.Copy), reads=["state"], writes=["stateT"])
                    if main:
                        for c in range(8):
                            S.add("pe", lambda e, c=c: e.transpose(out=tp[:, c * 128:(c + 1) * 128], in_=ytok[:, c * 128:(c + 1) * 128], identity=ident), reads=["ytok", "cmb"], writes=["tp"])
                        dve(lambda e: e.tensor_tensor(out=ygT[:, :, cols], in0=tp[:].rearrange("p (c t) -> p c t", t=128), in1=szT[:, :, cols], op=ALU.mult),
                            ["tp", zk], ["ygT"])

                for q in range(T // 128):
                    chunk(q)
                fdrip(100000)
                if main:
                    drip(100000)
                    S.add("act", lambda e: e.activation(out=sq[:], in_=ygT[:], func=AF.Square), reads=["ygT"], writes=["hT"])
                    for g in range(2):
                        for cc in range(4):
                            S.add("pe", lambda e, g=g, cc=cc: e.matmul(stp[:, g * T:(g + 1) * T], lhsT=ones, rhs=sq[:, g * 4 + cc, :], start=(cc == 0), stop=(cc == 3)),
                                  reads=["hT", "cmb"], writes=["stp"])
                    rg = [rstd, mean_b]
                    rgk = ["rstd", "mean_b"]
                    for g in range(2):
                        S.add("act", lambda e, g=g: e.activation(out=tmpa[:], in_=stp[:, g * T:(g + 1) * T], func=AF.Ln, scale=1.0 / 512, bias=epsr[:, 0:1]), reads=["stp", "eps"], writes=["tmpa"])
                        S.add("act", lambda e, g=g: e.activation(out=rg[g][:], in_=tmpa[:], func=AF.Exp, scale=-0.5), reads=["tmpa"], writes=[rgk[g]])
                    for c in range(8):
                        S.add("dve", lambda e, c=c: e.scalar_tensor_tensor(out=cat[:, 8 + c, :], in0=ygT[:, c, :], scalar=cvs("ng", c), in1=rg[c // 4][:], op0=ALU.mult, op1=ALU.mult),
                              reads=["ygT", "cv", rgk[c // 4]], writes=[("cat", 8 + c)])
                    S.add("act", lambda e: e.activation(out=sq[:], in_=acc[:], func=AF.Copy), reads=["acc"], writes=["hT"])
                    for c in range(8):
                        S.add("pe", lambda e, c=c: e.matmul(stp[:, 0:T], lhsT=ones, rhs=sq[:, c, :], start=(c == 0), stop=(c == 7)), reads=["hT", "cmb"], writes=["stp"])
                    S.add("act", lambda e: e.activation(out=mean_b[:], in_=stp[:, 0:T], func=AF.Copy, scale=1.0 / D), reads=["stp"], writes=["mean_b"])
                    S.add("act", lambda e: e.activation(out=sq[:], in_=acc[:], func=AF.Square), reads=["acc"], writes=["hT"])
                    for c in range(8):
                        S.add("pe", lambda e, c=c: e.matmul(stp[:, T:2 * T], lhsT=ones, rhs=sq[:, c, :], start=(c == 0), stop=(c == 7)), reads=["hT", "cmb"], writes=["stp"])
                    S.add("dve", lambda e: e.tensor_tensor(out=tmpa[:], in0=mean_b[:], in1=mean_b[:], op=ALU.mult), reads=["mean_b"], writes=["tmpa"])
                    S.add("dve", lambda e: e.scalar_tensor_tensor(out=tmpb[:], in0=stp[:, T:2 * T], scalar=1.0 / D, in1=tmpa[:], op0=ALU.mult, op1=ALU.subtract),
                          reads=["stp", "tmpa"], writes=["tmpb"])
                    S.add("act", lambda e: e.activation(out=tmpa[:], in_=tmpb[:], func=AF.Ln, bias=epsr[:, 1:2]), reads=["tmpb", "eps"], writes=["tmpa"])
                    S.add("act", lambda e: e.activation(out=rstd[:], in_=tmpa[:], func=AF.Exp, scale=-0.5), reads=["tmpa"], writes=["rstd"])
                    for c in range(8):
                        S.add("dve", lambda e, c=c: e.tensor_tensor(out=acc[:, c, :], in0=acc[:, c, :], in1=mean_b[:], op=ALU.subtract), reads=[("acc", c), "mean_b"], writes=[("acc", c)])
                        S.add("dve", lambda e, c=c: e.scalar_tensor_tensor(out=acc[:, c, :], in0=acc[:, c, :], scalar=cvs("ln_g", c), in1=rstd[:], op0=ALU.mult, op1=ALU.mult),
                              reads=[("acc", c), "rstd", "cv"], writes=[("acc", c)])
                        S.add("act", lambda e, c=c: e.activation(out=cat[:, c, :], in_=acc[:, c, :], func=AF.Silu, bias=cvs("ln_b", c)), reads=[("acc", c), "cv"], writes=[("cat", c)])
                    S.add("sp", lambda e: e.dma_start(out=CAT_v[:, :, (i - nt) * T:(i - nt + 1) * T], in_=cat[:]), reads=["cat"], writes=[("CAT", i - nt)], dma="catst")

            for _ in frontA(0):
                pass
            for i in range(2 * nt):
                fgen = frontA(i + 1) if i + 1 < 2 * nt else iter(())
                backA(i, fgen)
                if i == nt - 1:
                    S.add("dve", lambda e: e.tensor_scalar(out=state[:], in0=state[:], scalar1=cvs("flag"), scalar2=None, op0=ALU.mult), reads=["state", "cv"], writes=["state"])
                    S.add("act", lambda e: e.activation(out=stateT[:], in_=state[:], func=AF.Copy), reads=["state"], writes=["stateT"])
            S.barrier()
            S.emit_phase()


        Aall = sb("Aall", [128, NCHK, NE], F32)
        Wall = sb("Wall", [128, NCHK, NE], F32)
        Pall = sb("Pall", [128, NCHK, NE], F32)
        cnt = sb("cnt", [128, NE], F32)
        S.add("dve", lambda e: e.memset(cnt[:], 0.0), writes=["cnt"])
        build_bc(nc, S, nt, sb, ps, cv, cvs, cm, cmb, epsr, Aall, Wall, Pall, cnt, bc3, bcm,
                 dict(CAT_v=CAT_v, xT_v=xT_v, w_out_d=w_out_d, memT_d=memT_d, w_q_d=w_q_d, w_k_d=w_k_d, w_v_d=w_v_d, w_o_d=w_o_d, w_r_d=w_r_d, gbc_d=gbc_d,
                      X2_d=X2_d, H3_d=H3_d, Xs_d=Xs_d, Ys_d=Ys_d, out_d=out_d, w_gate_d=w_gate_d, w_up_d=w_up_d, w_down_d=w_down_d,
                      NSLOT_BLKS=NSLOT_BLKS, NCHK=NCHK, ereg=ereg_top), dbg, dbg_d)
    return nc


def build_bc(nc, S, nt, sb, ps, cv, cvs, cm, cmb, epsr, Aall, Wall, Pall, cnt, bc3, bcm, dd, dbg, dbg_d):
    CAT_v = dd["CAT_v"]
    xT_v = dd["xT_v"]
    NCHK = dd["NCHK"]
    NB = dd["NSLOT_BLKS"]
    identf = cm[:, 0:128]
    ident = cmb[:, 0:128]
    ones = cmb[:, 128:256]
    stri_lt = cmb[:, 256:384]
    with contextlib.ExitStack() as pb_:
        B = lambda n, s_, d: sb(n, s_, d, pb_)
        wq = B("wq", [128, 8, D], BF16)
        w_outT = B("w_outT", [128, 16, D], BF16)
        catb = B("catb", [128, 16, T], BF16)
        wo = B("wo", [128, 8, D], BF16)
        wkv = B("wkv", [128, 8, D], BF16)
        wv = B("wv", [128, 8, D], BF16)
        kT = B("kT", [128, 8, 256], BF16)
        vtok = B("vtok", [128, 2, D], BF16)
        wr = B("wr", [128, 8, 36], F32)
        gmo = B("gmo", [128, D], F32)
        memT = B("memTs", [128, 8, 256], F32)
        mnT = B("mnT", [128, 8, 256], BF16)
        xts = [B(f"xtb{i}", [128, 8, T], F32) for i in range(2)]
        tmpa2 = B("tmpa2", [128, T], F32)
        sq = B("sqb", [128, 8, T], BF16)
        hT = B("hTb", [128, 8, T], BF16)
        tmpa = B("tmpab", [128, T], F32)
        rstd = B("rstdb", [128, T], F32)
        rinv = B("rinv", [128, T], F32)
        qTs = [B(f"qT{i}", [128, 8, T], BF16) for i in range(2)]
        pT = B("pT", [128, 2, T], BF16)
        oT = B("oT", [128, 8, T], BF16)
        x2tok = B("x2tok", [128, D], F32)
        junk = B("junk", [128, D], F32)
        h3tok = B("h3tok", [128, D], BF16)
        r1 = B("r1", [128, 16], F32)
        lg = B("lg", [128, 36], F32)
        lm = B("lm", [128, 32], F32)
        ex = B("ex", [128, 32], F32)
        gm = B("gm", [128, 8], F32)
        m8 = B("m8", [128, 8], F32)
        Abf = B("Abf", [128, 32], BF16)
        mm = [ps(f"mmb{i}", [128, 512], F32, pb_) for i in range(2)]
        stp = ps("stpb", [128, 512], F32, pb_)
        tp2 = ps("tp2", [128, 1024], F32, pb_)
        nmm = [0]

        def nextmm():
            pbk = mm[nmm[0] % 2]
            key = f"mmb{nmm[0] % 2}"
            nmm[0] += 1
            return pbk, key

        def rms_h(src, skey, gname, dst, dkey, n):
            S.add("act", lambda e: e.activation(out=sq[:, :, 0:n], in_=src[:, :, 0:n], func=AF.Square), reads=[skey], writes=["sqb"])
            pbk, key = nextmm()
            for k in range(8):
                S.add("pe", lambda e, k=k: e.matmul(pbk[:, 0:n], lhsT=ones, rhs=sq[:, k, 0:n], start=(k == 0), stop=(k == 7)), reads=["sqb", "cmb"], writes=[key])
            S.add("act", lambda e: e.activation(out=tmpa[:, 0:n], in_=pbk[:, 0:n], func=AF.Ln, scale=1.0 / D, bias=epsr[:, 0:1]), reads=[key, "eps"], writes=["tmpab"])
            S.add("act", lambda e: e.activation(out=rstd[:, 0:n], in_=tmpa[:, 0:n], func=AF.Exp, scale=-0.5), reads=["tmpab"], writes=["rstdb"])
            for k in range(8):
                S.add("dve", lambda e, k=k: e.scalar_tensor_tensor(out=dst[:, k, 0:n], in0=src[:, k, 0:n], scalar=cvs(gname, k), in1=rstd[:, 0:n], op0=ALU.mult, op1=ALU.mult),
                      reads=[skey, "cv", "rstdb"], writes=[(dkey, k)])

        def wload(dst, key, src_d):
            v = src_d.rearrange("(ko p) n -> p ko n", p=128)
            for k in range(8):
                S.add("pool", lambda e, k=k: e.dma_start(out=dst[:, k, :], in_=v[:, k, :]), writes=[(key, k)], dma=key)

        zt = B("zt", [128, D], BF16)
        S.add("pool", lambda e: e.memset(zt[:], 0.0), writes=["zt"])
        NR = dd["NSLOT_BLKS"] * BLK // 128
        xs_v = dd["Xs_d"].rearrange("(r p) d -> p r d", p=128)
        for zr0 in range(0, NR, 16):
            zr1 = min(NR, zr0 + 16)
            S.add("sp", lambda e, zr0=zr0, zr1=zr1: e.dma_start(out=xs_v[:, zr0:zr1, :], in_=zt[:].unsqueeze(1).to_broadcast((128, zr1 - zr0, D))), reads=["zt"], writes=["Xs"], dma="xsz")
        wout_v = dd["w_out_d"].rearrange("(ko p) n -> p ko n", p=128)
        for k in range(16):
            S.add("pool", lambda e, k=k: e.dma_start(out=w_outT[:, k, :], in_=wout_v[:, k, :]), writes=[("w_outT", k)], dma="w_out")
        wload(wq, "wq", dd["w_q_d"])
        wload(wo, "wo", dd["w_o_d"])
        wload(wkv, "wkv", dd["w_k_d"])
        S.add("sp", lambda e: e.dma_start(out=memT[:], in_=dd["memT_d"].rearrange("(ko p) t -> p ko t", p=128)), writes=["memTs"], dma="memT")
        S.add("sp", lambda e: e.dma_start(out=wr[:], in_=dd["w_r_d"].rearrange("(ko p) n -> p ko n", p=128)), writes=["wr"], dma="wr")
        S.add("sp", lambda e: e.dma_start(out=gmo[:], in_=dd["gbc_d"][:, 0:D]), writes=["gmo"], dma="gmo")
        for k in range(8):
            S.add("dve", lambda e, k=k: e.tensor_scalar(out=wr[:, k, :], in0=wr[:, k, :], scalar1=cvs("g_moe", k), scalar2=None, op0=ALU.mult), reads=["wr", "cv"], writes=["wr"])
        rms_h(memT, "memTs", "g_mem", mnT, "mnT", 256)
        for j in range(8):
            pbk, key = nextmm()
            for k in range(8):
                S.add("pe", lambda e, k=k, j=j, pbk=pbk: e.matmul(pbk[:, 0:256], lhsT=wkv[:, k, j * 128:(j + 1) * 128], rhs=mnT[:, k, :], start=(k == 0), stop=(k == 7)),
                      reads=["wkv", "mnT"], writes=[key])
            S.add("act", lambda e, j=j, pbk=pbk: e.activation(out=kT[:, j, :], in_=pbk[:, 0:256], func=AF.Copy), reads=[key], writes=[("kT", j)])
        wload(wv, "wv", dd["w_v_d"])
        for jb in range(2):
            for dh in range(2):
                pbk, key = nextmm()
                for k in range(8):
                    S.add("pe", lambda e, k=k, jb=jb, dh=dh, pbk=pbk: e.matmul(pbk[:, 0:512], lhsT=mnT[:, k, jb * 128:(jb + 1) * 128], rhs=wv[:, k, dh * 512:(dh + 1) * 512], start=(k == 0), stop=(k == 7)),
                          reads=["wv", "mnT"], writes=[key])
                S.add("act", lambda e, jb=jb, dh=dh, pbk=pbk: e.activation(out=vtok[:, jb, dh * 512:(dh + 1) * 512], in_=pbk[:, 0:512], func=AF.Copy), reads=[key], writes=[("vtok", jb * 2 + dh)])

        def frontB(i):
            par = i % 2
            xt, xk = xts[par], f"xtb{par}"
            qT, qk = qTs[par], f"qT{par}"
            S.add("sp", lambda e: e.dma_start(out=xt[:], in_=xT_v[:, :, (nt + i) * T:(nt + i + 1) * T]), writes=[xk], dma=f"xtb{par}")
            S.add("sp", lambda e: e.dma_start(out=catb[:], in_=CAT_v[:, :, i * T:(i + 1) * T]), reads=[("CAT", i)], writes=["catb"], dma="catb")
            for dz in range(8):
                pbk, key = nextmm()
                for k in range(16):
                    S.add("pe", lambda e, k=k, dz=dz, pbk=pbk: e.matmul(pbk[:, 0:T], lhsT=w_outT[:, k, dz * 128:(dz + 1) * 128], rhs=catb[:, k, :], start=(k == 0), stop=(k == 15)),
                          reads=["w_outT", "catb"], writes=[key])
                S.add("dve", lambda e, dz=dz, pbk=pbk: e.tensor_tensor(out=xt[:, dz, :], in0=xt[:, dz, :], in1=pbk[:, 0:T], op=ALU.add), reads=[xk, key], writes=[xk])
                yield
            rms_h(xt, xk, "g_xa", hT, "hTb", T)
            yield
            for j in range(8):
                pbk, key = nextmm()
                for k in range(8):
                    S.add("pe", lambda e, k=k, j=j, pbk=pbk: e.matmul(pbk[:, 0:T], lhsT=wq[:, k, j * 128:(j + 1) * 128], rhs=hT[:, k, :], start=(k == 0), stop=(k == 7)), reads=["wq", "hTb"], writes=[key])
                S.add("act", lambda e, j=j, pbk=pbk: e.activation(out=qT[:, j, :], in_=pbk[:, 0:T], func=AF.Copy), reads=[key], writes=[(qk, j)])
                yield

        def backB(i, fgen):
            par = i % 2
            xt, xk = xts[par], f"xtb{par}"
            qT, qk = qTs[par], f"qT{par}"

            def fdrip(n):
                for _ in range(n):
                    if next(fgen, "end") == "end":
                        break

            for h in range(4):
                fdrip(2)
                for jb in range(2):
                    pbk, key = nextmm()
                    for dc in range(2):
                        S.add("pe", lambda e, h=h, jb=jb, dc=dc, pbk=pbk: e.matmul(pbk[:, 0:T], lhsT=kT[:, h * 2 + dc, jb * 128:(jb + 1) * 128], rhs=qT[:, h * 2 + dc, :], start=(dc == 0), stop=(dc == 1)),
                              reads=["kT", (qk, h * 2 + dc)], writes=[key])
                    S.add("act", lambda e, jb=jb, pbk=pbk: e.activation(out=pT[:, jb, :], in_=pbk[:, 0:T], func=AF.Exp, scale=1.0 / 16.0), reads=[key], writes=[("pT", jb)])
                for jb in range(2):
                    S.add("pe", lambda e, jb=jb: e.matmul(stp[:, 0:T], lhsT=ones, rhs=pT[:, jb, :], start=(jb == 0), stop=(jb == 1)), reads=[("pT", jb), "cmb"], writes=["stpb"])
                S.add("act", lambda e: e.activation(out=tmpa2[:], in_=stp[:, 0:T], func=AF.Ln), reads=["stpb"], writes=["tmpa2"])
                S.add("act", lambda e: e.activation(out=rinv[:], in_=tmpa2[:], func=AF.Exp, scale=-1.0), reads=["tmpa2"], writes=["rinv"])
                for dvc in range(2):
                    pbk, key = nextmm()
                    for jb in range(2):
                        S.add("pe", lambda e, h=h, jb=jb, dvc=dvc, pbk=pbk: e.matmul(pbk[:, 0:T], lhsT=vtok[:, jb, h * 256 + dvc * 128:h * 256 + (dvc + 1) * 128], rhs=pT[:, jb, :], start=(jb == 0), stop=(jb == 1)),
                              reads=["vtok", ("pT", jb)], writes=[key])
                    S.add("dve", lambda e, h=h, dvc=dvc, pbk=pbk: e.tensor_tensor(out=oT[:, h * 2 + dvc, :], in0=pbk[:, 0:T], in1=rinv[:], op=ALU.mult), reads=[key, "rinv"], writes=[("oT", h * 2 + dvc)])
            for dz in range(8):
                fdrip(1)
                pbk, key = nextmm()
                for k in range(8):
                    S.add("pe", lambda e, k=k, dz=dz, pbk=pbk: e.matmul(pbk[:, 0:T], lhsT=wo[:, k, dz * 128:(dz + 1) * 128], rhs=oT[:, k, :], start=(k == 0), stop=(k == 7)), reads=["wo", "oT"], writes=[key])
                S.add("dve", lambda e, dz=dz, pbk=pbk: e.tensor_tensor(out=xt[:, dz, :], in0=xt[:, dz, :], in1=pbk[:, 0:T], op=ALU.add), reads=[xk, key], writes=[xk])

            def chunkB(q):
                fdrip(1)
                ci = i * (T // 128) + q
                cols = slice(q * 128, (q + 1) * 128)
                rows = slice(ci * 128, (ci + 1) * 128)
                for c in range(8):
                    S.add("pe", lambda e, c=c: e.transpose(out=tp2[:, c * 128:(c + 1) * 128], in_=xt[:, c, cols], identity=identf), reads=[xk, "cm"], writes=["tp2"])
                S.add("act", lambda e: e.activation(out=x2tok[:], in_=tp2[:], func=AF.Copy), reads=["tp2"], writes=["x2tok"])
                S.add("sp", lambda e: e.dma_start(out=dd["X2_d"][rows, :], in_=x2tok[:]), reads=["x2tok"], writes=[("X2", ci)], dma="x2st")
                S.add("act", lambda e: e.activation(out=junk[:], in_=x2tok[:], func=AF.Square, accum_out=r1[:, 0:1]), reads=["x2tok"], writes=["junk", ("r1", 0)])
                S.add("act", lambda e: e.activation(out=r1[:, 1:2], in_=r1[:, 0:1], func=AF.Ln, scale=1.0 / D, bias=epsr[:, 0:1]), reads=[("r1", 0), "eps"], writes=[("r1", 1)])
                S.add("act", lambda e: e.activation(out=r1[:, 2:3], in_=r1[:, 1:2], func=AF.Exp, scale=-0.5), reads=[("r1", 1)], writes=[("r1", 2)])
                rs3 = r1[:, 2:3]
                S.add("dve", lambda e: e.scalar_tensor_tensor(out=h3tok[:], in0=x2tok[:], scalar=rs3, in1=gmo[:], op0=ALU.mult, op1=ALU.mult), reads=["x2tok", ("r1", 2), "gmo"], writes=["h3tok"])
                S.add("sp", lambda e: e.dma_start(out=dd["H3_d"][rows, :], in_=h3tok[:]), reads=["h3tok"], writes=[("H3", ci)], dma="h3st")
                for k in range(8):
                    S.add("pe", lambda e, k=k: e.matmul(stp[:, 0:36], lhsT=xt[:, k, cols], rhs=wr[:, k, :], start=(k == 0), stop=(k == 7)), reads=[xk, "wr"], writes=["stpb"])
                S.add("dve", lambda e: e.scalar_tensor_tensor(out=lg[:], in0=stp[:, 0:36], scalar=rs3, in1=cvs("rb", 0, 36), op0=ALU.mult, op1=ALU.add), reads=["stpb", ("r1", 2), "cv"], writes=["lg"])
                S.add("dve", lambda e: e.tensor_reduce(out=r1[:, 3:4], in_=lg[:, 0:4], axis=AX.X, op=ALU.max), reads=["lg"], writes=[("r1", 3)])
                S.add("dve", lambda e: e.tensor_scalar(out=r1[:, 4:5], in0=r1[:, 3:4], scalar1=-1.0, scalar2=None, op0=ALU.mult), reads=[("r1", 3)], writes=[("r1", 4)])
                S.add("act", lambda e: e.activation(out=gm[:, 0:4], in_=lg[:, 0:4], func=AF.Exp, bias=r1[:, 4:5], accum_out=r1[:, 5:6]), reads=["lg", ("r1", 4)], writes=[("gm", 0), ("r1", 5)])
                S.add("dve", lambda e: e.reciprocal(out=r1[:, 6:7], in_=r1[:, 5:6]), reads=[("r1", 5)], writes=[("r1", 6)])
                S.add("dve", lambda e: e.tensor_scalar(out=gm[:, 4:8], in0=lg[:, 0:4], scalar1=r1[:, 3:4], scalar2=None, op0=ALU.is_equal), reads=["lg", ("r1", 3)], writes=[("gm", 1)])
                S.add("dve", lambda e: e.tensor_scalar(out=gm[:, 4:8], in0=gm[:, 4:8], scalar1=1.0e4, scalar2=-1.0e4, op0=ALU.mult, op1=ALU.add), reads=[("gm", 1)], writes=[("gm", 1)])
                S.add("dve", lambda e: e.tensor_tensor(out=lm[:].rearrange("p (g e) -> p g e", e=8), in0=lg[:, 4:36].rearrange("p (g e) -> p g e", e=8), in1=bc3(gm[:, 4:8], 8), op=ALU.add),
                      reads=["lg", ("gm", 1)], writes=["lm"])
                S.add("dve", lambda e: e.max(out=m8[:], in_=lm[:]), reads=["lm"], writes=["m8"])
                S.add("dve", lambda e: e.tensor_scalar(out=r1[:, 7:8], in0=m8[:, 0:1], scalar1=-1.0, scalar2=None, op0=ALU.mult), reads=["m8"], writes=[("r1", 7)])
                S.add("dve", lambda e: e.tensor_scalar(out=Aall[:, ci, :], in0=lm[:], scalar1=m8[:, 1:2], scalar2=None, op0=ALU.is_ge), reads=["lm", "m8"], writes=[("Aall", ci)])
                S.add("act", lambda e: e.activation(out=ex[:], in_=lm[:], func=AF.Exp, bias=r1[:, 7:8]), reads=["lm", ("r1", 7)], writes=["ex"])
                S.add("dve", lambda e: e.tensor_tensor(out=ex[:], in0=ex[:], in1=Aall[:, ci, :], op=ALU.mult), reads=["ex", ("Aall", ci)], writes=["ex"])
                S.add("dve", lambda e: e.tensor_reduce(out=r1[:, 8:9], in_=ex[:], axis=AX.X, op=ALU.add), reads=["ex"], writes=[("r1", 8)])
                S.add("dve", lambda e: e.reciprocal(out=r1[:, 9:10], in_=r1[:, 8:9]), reads=[("r1", 8)], writes=[("r1", 9)])
                S.add("dve", lambda e: e.tensor_tensor(out=r1[:, 10:11], in0=r1[:, 9:10], in1=r1[:, 6:7], op=ALU.mult), reads=[("r1", 9), ("r1", 6)], writes=[("r1", 10)])
                S.add("dve", lambda e: e.tensor_scalar(out=Wall[:, ci, :], in0=ex[:], scalar1=r1[:, 10:11], scalar2=None, op0=ALU.mult), reads=["ex", ("r1", 10)], writes=[("Wall", ci)])
                S.add("pool", lambda e: e.tensor_copy(out=Abf[:], in_=Aall[:, ci, :]), reads=[("Aall", ci)], writes=["Abf"])
                S.add("pe", lambda e: e.matmul(stp[:, 64:96], lhsT=stri_lt, rhs=Abf[:], start=True, stop=True), reads=["Abf", "cmb"], writes=["stpb"])
                S.add("pe", lambda e: e.matmul(stp[:, 96:128], lhsT=ones, rhs=Abf[:], start=True, stop=True), reads=["Abf", "cmb"], writes=["stpb"])
                S.add("dve", lambda e: e.tensor_tensor(out=Pall[:, ci, :], in0=stp[:, 64:96], in1=cnt[:], op=ALU.add), reads=["stpb", "cnt"], writes=[("Pall", ci)])
                S.add("dve", lambda e: e.tensor_tensor(out=cnt[:], in0=cnt[:], in1=stp[:, 96:128], op=ALU.add), reads=["stpb", "cnt"], writes=["cnt"])

            for q in range(T // 128):
                chunkB(q)
            fdrip(100000)

        for _ in frontB(0):
            pass
        for i in range(nt):
            backB(i, frontB(i + 1) if i + 1 < nt else iter(()))
        S.barrier()
        S.emit_phase()

    with contextlib.ExitStack() as pc_:
        C = lambda n, s_, d: sb(n, s_, d, pc_)
        gfin = C("gfin", [128, D], F32)
        S.add("sp", lambda e: e.dma_start(out=gfin[:], in_=dd["gbc_d"][:, D:2 * D]), writes=["gfin"], dma="gfin")
        nblk = C("nblk", [128, NE], F32)
        cA = C("cA", [128, NE], F32)
        cB = C("cB", [128, NE], F32)
        sslot = C("sslot", [128, NE], F32)
        shi = C("shi", [128, NCHK], F32)
        slo = C("slo", [128, NCHK], F32)
        whi = C("whi", [128, NCHK], F32)
        wlo = C("wlo", [128, NCHK], F32)
        shi_i = C("shi_i", [128, NCHK], I32)
        slo_i = C("slo_i", [128, NCHK], I32)
        eb = C("eb", [128, NB], F32)
        ebi = C("ebi", [128, NB], I32)
        widf = C("widf", [128, NB], F32)
        widx0 = C("widx0", [128, NB], I32)
        rr = C("rr", [128, 2, 4], F32)
        c1 = contextlib.ExitStack()
        C1 = lambda n, s_, d: sb(n, s_, d, c1)
        cmp = C1("cmp", [128, NE, 16], F32)
        t1 = C1("t1c", [128, NCHK, NE], F32)
        sd = C1("sd", [128, NCHK, NE], F32)
        t2 = C1("t2c", [128, NCHK, NE], F32)
        cmpb = C1("cmpb", [128, NB, NE], F32)
        S.add("dve", lambda e: e.tensor_tensor(out=cmp[:], in0=bc3(cnt[:], 16), in1=bcm(cvs("thr", 0, 16), NE), op=ALU.is_gt), reads=["cnt", "cv"], writes=["cmp"])
        S.add("dve", lambda e: e.tensor_reduce(out=nblk[:], in_=cmp[:], axis=AX.X, op=ALU.add), reads=["cmp"], writes=["nblk"])
        S.add("dve", lambda e: e.tensor_copy(out=cA[:], in_=nblk[:]), reads=["nblk"], writes=["cA"])
        cur, oth, ck, ok_ = cA, cB, "cA", "cB"
        for sh in (1, 2, 4, 8, 16):
            S.add("dve", lambda e, cur=cur, oth=oth, sh=sh: e.tensor_copy(out=oth[:, 0:sh], in_=cur[:, 0:sh]), reads=[ck], writes=[ok_])
            S.add("dve", lambda e, cur=cur, oth=oth, sh=sh: e.tensor_tensor(out=oth[:, sh:NE], in0=cur[:, sh:NE], in1=cur[:, 0:NE - sh], op=ALU.add), reads=[ck], writes=[ok_])
            cur, oth, ck, ok_ = oth, cur, ok_, ck
        endb, endk = cur, ck
        S.add("dve", lambda e: e.tensor_tensor(out=sslot[:], in0=endb[:], in1=nblk[:], op=ALU.subtract), reads=[endk, "nblk"], writes=["sslot"])
        S.add("dve", lambda e: e.tensor_scalar(out=sslot[:], in0=sslot[:], scalar1=float(BLK), scalar2=None, op0=ALU.mult), reads=["sslot"], writes=["sslot"])
        S.add("dve", lambda e: e.tensor_tensor(out=t1[:], in0=Pall[:], in1=bcm(sslot[:], NCHK), op=ALU.add), reads=["Pall", "sslot"], writes=["t1c"])
        S.add("dve", lambda e: e.scalar_tensor_tensor(out=sd[:].rearrange("p c e -> p (c e)"), in0=t1[:].rearrange("p c e -> p (c e)"), scalar=1.0, in1=Aall[:].rearrange("p c e -> p (c e)"), op0=ALU.add, op1=ALU.mult),
              reads=["t1c", "Aall"], writes=["sd"])
        S.add("dve", lambda e: e.tensor_scalar(out=sd[:], in0=sd[:], scalar1=-1.0, scalar2=None, op0=ALU.add), reads=["sd"], writes=["sd"])
        S.add("dve", lambda e: e.tensor_reduce(out=shi[:], in_=sd[:], axis=AX.X, op=ALU.max), reads=["sd"], writes=["shi"])
        S.add("dve", lambda e: e.tensor_scalar(out=t2[:], in0=t1[:], scalar1=-1.0, scalar2=BIGS, op0=ALU.mult, op1=ALU.add), reads=["t1c"], writes=["t2c"])
        S.add("dve", lambda e: e.tensor_tensor(out=t2[:], in0=t2[:], in1=Aall[:], op=ALU.mult), reads=["t2c", "Aall"], writes=["t2c"])
        S.add("dve", lambda e: e.tensor_reduce(out=slo[:], in_=t2[:], axis=AX.X, op=ALU.max), reads=["t2c"], writes=["slo"])
        S.add("dve", lambda e: e.tensor_scalar(out=slo[:], in0=slo[:], scalar1=-1.0, scalar2=BIGS, op0=ALU.mult, op1=ALU.add), reads=["slo"], writes=["slo"])
        S.add("dve", lambda e: e.tensor_tensor(out=t2[:], in0=sd[:], in1=bc3(shi[:], NE), op=ALU.is_equal), reads=["sd", "shi"], writes=["t2c"])
        S.add("dve", lambda e: e.tensor_tensor(out=t2[:], in0=t2[:], in1=Wall[:], op=ALU.mult), reads=["t2c", "Wall"], writes=["t2c"])
        S.add("dve", lambda e: e.tensor_reduce(out=whi[:], in_=t2[:], axis=AX.X, op=ALU.add), reads=["t2c"], writes=["whi"])
        S.add("dve", lambda e: e.tensor_reduce(out=wlo[:], in_=Wall[:], axis=AX.X, op=ALU.add), reads=["Wall"], writes=["wlo"])
        S.add("dve", lambda e: e.tensor_tensor(out=wlo[:], in0=wlo[:], in1=whi[:], op=ALU.subtract), reads=["wlo", "whi"], writes=["wlo"])
        S.add("dve", lambda e: e.tensor_copy(out=shi_i[:], in_=shi[:]), reads=["shi"], writes=["shi_i"])
        S.add("dve", lambda e: e.tensor_copy(out=slo_i[:], in_=slo[:]), reads=["slo"], writes=["slo_i"])
        S.add("dve", lambda e: e.tensor_tensor(out=cmpb[:], in0=bcm(endb[:], NB), in1=bc3(cvs("iob", 0, NB), NE), op=ALU.is_le), reads=[endk, "cv"], writes=["cmpb"])
        S.add("dve", lambda e: e.tensor_reduce(out=eb[:], in_=cmpb[:], axis=AX.X, op=ALU.add), reads=["cmpb"], writes=["eb"])
        S.add("dve", lambda e: e.tensor_scalar(out=widf[:], in0=eb[:], scalar1=256.0, scalar2=cvs("pid2"), op0=ALU.mult, op1=ALU.add), reads=["eb", "cv"], writes=["widf"])
        S.add("dve", lambda e: e.tensor_scalar(out=widf[:], in0=widf[:], scalar1=0.5, scalar2=None, op0=ALU.mult), reads=["widf"], writes=["widf"])
        S.add("dve", lambda e: e.tensor_copy(out=widx0[:], in_=widf[:]), reads=["widf"], writes=["widx"])

        c1.close()
        S.barrier()
        c2 = contextlib.ExitStack()
        C = lambda n, s_, d: sb(n, s_, d, c2)
        hbuf = [C(f"hbuf{i}", [128, D], BF16) for i in range(2)]
        for ci in range(NCHK):
            hb, hk = hbuf[ci % 2], f"hbuf{ci % 2}"
            S.add("sp", lambda e, ci=ci, hb=hb: e.dma_start(out=hb[:], in_=dd["H3_d"][ci * 128:(ci + 1) * 128, :]), reads=[("H3", ci)], writes=[hk], dma=f"hl{ci % 2}")
            for kk, idx in enumerate((shi_i, slo_i)):
                S.add("pool", lambda e, ci=ci, hb=hb, idx=idx: e.indirect_dma_start(out=dd["Xs_d"], out_offset=bass.IndirectOffsetOnAxis(ap=idx[:, ci:ci + 1], axis=0), in_=hb[:], in_offset=None),
                      reads=[hk, "shi_i", "slo_i"], writes=[("Xs", ci * 2 + kk)], dma=f"sc{ci % 2}")

        wg = [C(f"wg{i}", [128, 8, 512], BF16) for i in range(2)]
        wu = [C(f"wu{i}", [128, 8, 512], BF16) for i in range(2)]
        wd = [C(f"wd{i}", [128, 4, D], BF16) for i in range(2)]
        wst = [C(f"wst{i}", [128, 3, 4096], F32) for i in range(2)]
        xs_tok = C("xs_tok", [128, 2, D], BF16)
        xsT = C("xsT", [128, 8, BLK], BF16)
        sgl = C("sgl", [128, BLK], F32)
        aT = C("aT", [128, 4, BLK], BF16)
        ysb = C("ysb", [128, 2, D], F32)
        mmc = [ps(f"mmc{i}", [128, 512], F32, pc_) for i in range(4)]
        tpc = ps("tpc", [128, 8, BLK], BF16, pc_)
        ereg = dd["ereg"]
        S.add("pool", lambda e: e.reg_mov(ereg, NE * 128 - 1))
        nm = [0]

        def nextc():
            p_ = mmc[nm[0] % 4]
            k_ = f"mmc{nm[0] % 4}"
            nm[0] += 1
            return p_, k_

        for b in range(NB):
            bb = b % 2

            st_ = wst[bb]
            for m_, src_d in enumerate((dd["w_gate_d"], dd["w_up_d"], dd["w_down_d"])):
                S.add("pool", lambda e, m_=m_, src_d=src_d, st_=st_, b=b: e.indirect_dma_start(
                    out=st_[:, m_, :], out_offset=None, in_=src_d.rearrange("(r h) f -> r (h f)", h=2),
                    in_offset=bass.IndirectOffsetOnAxis(ap=widx0[:, b:b + 1], axis=0), bounds_check=ereg, oob_is_err=False),
                      reads=["widx"], writes=[(f"wst{bb}", m_)], dma=f"wst{bb}m{m_}")
            S.add("act", lambda e, st_=st_, bb=bb: e.activation(out=wg[bb][:].rearrange("p a b -> p (a b)"), in_=st_[:, 0, :], func=AF.Copy), reads=[(f"wst{bb}", 0)], writes=[f"wg{bb}"])
            S.add("dve", lambda e, st_=st_, bb=bb: e.tensor_copy(out=wu[bb][:].rearrange("p a b -> p (a b)"), in_=st_[:, 1, :]), reads=[(f"wst{bb}", 1)], writes=[f"wu{bb}"])
            S.add("act", lambda e, st_=st_, bb=bb: e.activation(out=wd[bb][:, 0:2, :].rearrange("p a b -> p (a b)"), in_=st_[:, 2, 0:2048], func=AF.Copy), reads=[(f"wst{bb}", 2)], writes=[(f"wd{bb}", 0)])
            S.add("dve", lambda e, st_=st_, bb=bb: e.tensor_copy(out=wd[bb][:, 2:4, :].rearrange("p a b -> p (a b)"), in_=st_[:, 2, 2048:4096]), reads=[(f"wst{bb}", 2)], writes=[(f"wd{bb}", 1)])
            S.add("sp", lambda e, b=b: e.dma_start(out=xs_tok[:], in_=dd["Xs_d"][b * BLK:(b + 1) * BLK, :].rearrange("(r p) d -> p r d", p=128)), reads=["Xs"], writes=["xs_tok"], dma="xsl")
            for r in range(2):
                for k in range(8):
                    S.add("pe", lambda e, r=r, k=k: e.transpose(out=tpc[:, k, r * 128:(r + 1) * 128], in_=xs_tok[:, r, k * 128:(k + 1) * 128], identity=ident), reads=["xs_tok", "cmb"], writes=["tpc"])
            S.add("act", lambda e: e.activation(out=xsT[:].rearrange("p k s -> p (k s)"), in_=tpc[:].rearrange("p k s -> p (k s)"), func=AF.Copy), reads=["tpc"], writes=["xsT"])
            for fc in range(4):
                pg, kg = nextc()
                for k in range(8):
                    S.add("pe", lambda e, k=k, fc=fc, pg=pg, bb=bb: e.matmul(pg[:, 0:BLK], lhsT=wg[bb][:, k, fc * 128:(fc + 1) * 128], rhs=xsT[:, k, :], start=(k == 0), stop=(k == 7)), reads=[f"wg{bb}", "xsT"], writes=[kg])
                pu, ku = nextc()
                for k in range(8):
                    S.add("pe", lambda e, k=k, fc=fc, pu=pu, bb=bb: e.matmul(pu[:, 0:BLK], lhsT=wu[bb][:, k, fc * 128:(fc + 1) * 128], rhs=xsT[:, k, :], start=(k == 0), stop=(k == 7)), reads=[f"wu{bb}", "xsT"], writes=[ku])
                S.add("act", lambda e, pg=pg: e.activation(out=sgl[:], in_=pg[:, 0:BLK], func=AF.Silu), reads=[kg], writes=["sgl"])
                S.add("dve", lambda e, pu=pu, fc=fc: e.tensor_tensor(out=aT[:, fc, :], in0=pu[:, 0:BLK], in1=sgl[:], op=ALU.mult), reads=[ku, "sgl"], writes=[("aT", fc)])
            for half in range(2):
                for dh in range(2):
                    py, ky = nextc()
                    for fc in range(4):
                        S.add("pe", lambda e, fc=fc, half=half, dh=dh, py=py, bb=bb: e.matmul(py[:, 0:512], lhsT=aT[:, fc, half * 128:(half + 1) * 128], rhs=wd[bb][:, fc, dh * 512:(dh + 1) * 512], start=(fc == 0), stop=(fc == 3)),
                              reads=["aT", f"wd{bb}"], writes=[ky])
                    S.add("act", lambda e, half=half, dh=dh, py=py: e.activation(out=ysb[:, half, dh * 512:(dh + 1) * 512], in_=py[:, 0:512], func=AF.Copy), reads=[ky], writes=[("ysb", half * 2 + dh)])
            S.add("sp", lambda e, b=b: e.dma_start(out=dd["Ys_d"][b * BLK:(b + 1) * BLK, :].rearrange("(r p) d -> p r d", p=128), in_=ysb[:]), reads=["ysb"], writes=[("Ys", b)], dma="yst")

        c2.close()
        S.barrier()
        c3 = contextlib.ExitStack()
        C = lambda n, s_, d: sb(n, s_, d, c3)
        ya = [C(f"ya{i}", [128, D], F32) for i in range(2)]
        yb = [C(f"yb{i}", [128, D], F32) for i in range(2)]
        x2c = [C(f"x2c{i}", [128, D], F32) for i in range(2)]
        osb = [C(f"osb{i}", [128, D], F32) for i in range(2)]
        for ci in range(NCHK):
            p = ci % 2
            S.add("pool", lambda e, ci=ci, p=p: e.indirect_dma_start(out=ya[p][:], out_offset=None, in_=dd["Ys_d"], in_offset=bass.IndirectOffsetOnAxis(ap=shi_i[:, ci:ci + 1], axis=0)),
                  reads=["Ys", "shi_i"], writes=[f"ya{p}"], dma=f"ga{p}")
            S.add("pool", lambda e, ci=ci, p=p: e.indirect_dma_start(out=yb[p][:], out_offset=None, in_=dd["Ys_d"], in_offset=bass.IndirectOffsetOnAxis(ap=slo_i[:, ci:ci + 1], axis=0)),
                  reads=["Ys", "slo_i"], writes=[f"yb{p}"], dma=f"gb{p}")
            S.add("sp", lambda e, ci=ci, p=p: e.dma_start(out=x2c[p][:], in_=dd["X2_d"][ci * 128:(ci + 1) * 128, :]), reads=[("X2", ci)], writes=[f"x2c{p}"], dma=f"x2l{p}")
            S.add("dve", lambda e, ci=ci, p=p: e.scalar_tensor_tensor(out=x2c[p][:], in0=ya[p][:], scalar=whi[:, ci:ci + 1], in1=x2c[p][:], op0=ALU.mult, op1=ALU.add), reads=[f"ya{p}", f"x2c{p}", "whi"], writes=[f"x2c{p}"])
            S.add("dve", lambda e, ci=ci, p=p: e.scalar_tensor_tensor(out=x2c[p][:], in0=yb[p][:], scalar=wlo[:, ci:ci + 1], in1=x2c[p][:], op0=ALU.mult, op1=ALU.add), reads=[f"yb{p}", f"x2c{p}", "wlo"], writes=[f"x2c{p}"])
            S.add("act", lambda e, p=p: e.activation(out=osb[p][:], in_=x2c[p][:], func=AF.Square, accum_out=rr[:, p, 0:1]), reads=[f"x2c{p}"], writes=[f"osb{p}", ("rr", p)])
            S.add("act", lambda e, p=p: e.activation(out=rr[:, p, 1:2], in_=rr[:, p, 0:1], func=AF.Ln, scale=1.0 / D, bias=epsr[:, 0:1]), reads=[("rr", p), "eps"], writes=[("rr", p)])
            S.add("act", lambda e, p=p: e.activation(out=rr[:, p, 2:3], in_=rr[:, p, 1:2], func=AF.Exp, scale=-0.5), reads=[("rr", p)], writes=[("rr", p)])
            S.add("dve", lambda e, p=p: e.scalar_tensor_tensor(out=osb[p][:], in0=x2c[p][:], scalar=rr[:, p, 2:3], in1=gfin[:], op0=ALU.mult, op1=ALU.mult), reads=[f"x2c{p}", ("rr", p), "gfin"], writes=[f"osb{p}"])
            S.add("sp", lambda e, ci=ci, p=p: e.dma_start(out=dd["out_d"][ci * 128:(ci + 1) * 128, :], in_=osb[p][:]), reads=[f"osb{p}"], dma=f"ost{p}")
        S.emit_phase(final=True)
        c3.close()
    return None


def make_cm():
    i = np.arange(128)
    ident = np.eye(128, dtype=np.float32)
    ones = np.ones((128, 128), np.float32)
    tri_incl = (i[:, None] <= i[None, :]).astype(np.float32)
    stri_gt = (i[:, None] > i[None, :]).astype(np.float32)
    stri_lt = (i[:, None] < i[None, :]).astype(np.float32)
    return np.ascontiguousarray(np.concatenate([ident, ones, tri_incl, stri_gt, stri_lt], axis=1))


def make_cv(inp, flag):
    cvv = np.zeros((128, NCV), np.float32)

    def put(name, arr):
        o, w = CV[name]
        cvv[:, o:o + w] = arr

    put("g_mix", pk(inp["g_mix"][0], 8))
    cw = np.asarray(inp["conv_w"][0], np.float32)
    put("cw", cw.reshape(31, 8, 128).transpose(2, 1, 0).reshape(128, 8 * 31))
    put("cb", pk(inp["conv_b"][0], 8))
    put("ln_g", pk(inp["ln_g"][0], 8))
    put("ln_b", pk(inp["ln_b"][0], 8))
    w4 = np.asarray(inp["ssd_conv_w"][0], np.float32)
    put("w4", w4.reshape(4, 12, 128).transpose(2, 1, 0).reshape(128, 48))
    put("b4", pk(inp["ssd_conv_b"][0], 12))
    dtb = np.zeros((128, 1), np.float32)
    dtb[0:16, 0] = inp["dt_bias"][0]
    put("dt_bias", dtb)
    put("a_log", np.tile(np.asarray(inp["a_log"][0], np.float32)[None, :], (128, 1)))
    put("dsk", np.tile(np.asarray(inp["d_skip"][0], np.float32)[None, :], (128, 1)))
    put("ng", pk(inp["ssd_norm_g"][0], 8))
    put("g_xa", pk(inp["g_xattn"][0], 8))
    put("g_mem", pk(inp["g_mem"][0], 8))
    put("g_moe", pk(inp["g_moe"][0], 8))
    rb = np.concatenate([np.asarray(inp["b_router_group"][0]), np.asarray(inp["b_router_expert"][0])]).astype(np.float32)
    put("rb", np.tile(rb[None, :], (128, 1)))
    put("flag", np.full((128, 1), flag, np.float32))
    put("thr", np.tile((np.arange(16, dtype=np.float32) * BLK)[None, :], (128, 1)))
    put("iob", np.tile(np.arange(64, dtype=np.float32)[None, :], (128, 1)))
    put("pid2", (2.0 * np.arange(128, dtype=np.float32))[:, None])
    return cvv


def make_in_maps(inp, nt, ncores):
    NTOK = nt * T
    x = np.asarray(inp["x"], np.float32)
    mem = np.asarray(inp["mem"], np.float32)
    cm = make_cm()
    gbc = np.concatenate([np.tile(np.asarray(inp["g_moe"][0], np.float32)[None, :], (128, 1)),
                          np.tile(np.asarray(inp["g_final"], np.float32)[None, :], (128, 1))], axis=1)
    w_r = np.ascontiguousarray(np.concatenate([inp["w_router_group"][0], inp["w_router_expert"][0]], axis=1).astype(np.float32))
    shared = dict(cm=cm, gbc=np.ascontiguousarray(gbc), w_in=np.ascontiguousarray(inp["w_in"][0]), w_out=np.ascontiguousarray(inp["w_out"][0]),
                  w_q=np.ascontiguousarray(inp["w_q"][0]), w_k=np.ascontiguousarray(inp["w_k"][0]), w_v=np.ascontiguousarray(inp["w_v"][0]),
                  w_o=np.ascontiguousarray(inp["w_o"][0]), w_r=w_r, w_gate_r=np.ascontiguousarray(np.asarray(inp["w_gate"][0], np.float32).reshape(NE, 8, 128, 512).transpose(0, 2, 1, 3)).reshape(NE * 256, 2048),
                  w_up_r=np.ascontiguousarray(np.asarray(inp["w_up"][0], np.float32).reshape(NE, 8, 128, 512).transpose(0, 2, 1, 3)).reshape(NE * 256, 2048),
                  w_down_r=np.ascontiguousarray(np.asarray(inp["w_down"][0], np.float32).reshape(NE, 4, 128, D).transpose(0, 2, 1, 3)).reshape(NE * 256, 2048))
    maps = []
    for c in range(ncores):
        b, half = c // 2, c % 2
        xm = x[b, half * NTOK:(half + 1) * NTOK]
        xp = x[b, 0:NTOK] if half == 1 else np.zeros_like(xm)
        xT = np.ascontiguousarray(np.concatenate([xp, xm], axis=0).T)
        m = dict(shared)
        m["xT"] = xT
        m["memT"] = np.ascontiguousarray(mem[b].T)
        m["cv"] = make_cv(inp, float(half))
        maps.append(m)
    return maps


_NC_CACHE = {}


def kernel(**inputs):
    nt = 16
    if nt not in _NC_CACHE:
        _NC_CACHE[nt] = build(nt)
    nc = _NC_CACHE[nt]
    maps = make_in_maps(inputs, nt, 8)
    res = run_bass_kernel_spmd(nc, maps, core_ids=list(range(8)))
    NTOK = nt * T
    out = np.zeros((4, 2 * NTOK, D), np.float32)
    for c in range(8):
        out[c // 2, (c % 2) * NTOK:(c % 2 + 1) * NTOK] = res.results[c]["out"]
    return out
```

```python
import contextlib
import numpy as np
import concourse.bass as bass
import concourse.mybir as mybir
from concourse.bass_utils import run_bass_kernel_spmd

F32 = mybir.dt.float32
BF16 = mybir.dt.bfloat16
I32 = mybir.dt.int32
AF = mybir.ActivationFunctionType
ALU = mybir.AluOpType
AX = mybir.AxisListType

ENGS = ("pe", "act", "dve", "pool", "sp")
SEM_CHUNK = 30000


class Op:
    __slots__ = ("eng", "fn", "deps", "dma", "sig", "idx", "dman", "seq")

    def __init__(self, eng, fn, dma):
        self.eng = eng
        self.fn = fn
        self.deps = set()
        self.dma = dma
        self.sig = False
        self.idx = -1
        self.dman = 0
        self.seq = 0


class Sched:
    def __init__(self, nc):
        self.nc = nc
        self.ops = {e: [] for e in ENGS}
        self.lastw = {}
        self.readers = {}
        self.subs = {}
        self.dma_cnt = {}
        self.nops = 0
        self.limit = None
        self.bar = set()
        self.dma_last = {}
        self.marks = []
        self.skipped = 0

    def mark(self, name):
        self.marks.append((name, self.nops))

    def _expand(self, key, record):
        name, sub = key if isinstance(key, tuple) else (key, None)
        s = self.subs.setdefault(name, set())
        if sub is None:
            return [(name, None)] + [(name, x) for x in s]
        s.add(sub)
        if record:
            return [(name, sub)]
        return [(name, sub), (name, None)]

    def add(self, eng, fn, reads=(), writes=(), dma=None, force=False):
        if self.limit is not None and self.nops >= self.limit and not force:
            self.skipped += 1
            return None
        op = Op(eng, fn, dma)
        op.seq = self.nops
        self.nops += 1
        for k in reads:
            for kk in self._expand(k, False):
                w = self.lastw.get(kk)
                if w is not None:
                    op.deps.add(w)
        for k in writes:
            for kk in self._expand(k, False):
                w = self.lastw.get(kk)
                if w is not None:
                    op.deps.add(w)
                for r in self.readers.get(kk, ()):
                    op.deps.add(r)
        op.deps |= self.bar
        op.deps.discard(op)
        for k in reads:
            for kk in self._expand(k, True):
                lst = self.readers.setdefault(kk, [])
                if dma is None:
                    lst[:] = [r for r in lst if not (r.dma is None and r.eng == eng)]
                lst.append(op)
        for k in writes:
            for kk in self._expand(k, True):
                self.lastw[kk] = op
                self.readers[kk] = []
        if dma is not None:
            n = self.dma_cnt.get(dma, 0) + 1
            self.dma_cnt[dma] = n
            op.dman = n
            self.dma_last[dma] = op
        self.ops[eng].append(op)
        return op

    def barrier(self):
        deps = set(self.dma_last.values())
        for e in ENGS:
            for op in reversed(self.ops[e]):
                if op.dma is None:
                    deps.add(op)
                    op.sig = True
                    break
        self.bar = deps

    def emit_phase(self, final=False):
        nc = self.nc
        if not hasattr(self, "_st"):
            self._st = contextlib.ExitStack()
            self._esems = {e: [self._st.enter_context(nc.semaphore(f"s_{e}{i}")) for i in range(3)] for e in ENGS}
            self._dsems = {}
            self._start = {e: 0 for e in ENGS}
            self._cnt = {e: 0 for e in ENGS}
            self._waited = {e: {} for e in ENGS}
            self._emitted = set()
            self._prevbar = set()
        esems, dsems = self._esems, self._dsems
        cur = {e: self.ops[e][self._start[e]:] for e in ENGS}
        curset = set()
        for e in ENGS:
            curset.update(cur[e])
        for e in ENGS:
            for op in cur[e]:
                best = {}
                keep = set()
                for d in op.deps:
                    if d.dma is not None:
                        keep.add(d)
                        continue
                    if d not in curset and d not in self._prevbar:
                        continue
                    if d.eng == "pe" and op.eng == "pe" and op.dma is None:
                        continue
                    b = best.get(d.eng)
                    if b is None or d.seq > b.seq:
                        best[d.eng] = d
                keep.update(best.values())
                op.deps = keep
                for d in keep:
                    if d.dma is None:
                        assert d in curset or d.sig
                        d.sig = True
        for e in ENGS:
            for op in cur[e]:
                if op.dma is None and op.sig:
                    op.idx = self._cnt[e]
                    self._cnt[e] += 1
        for k in self.dma_cnt:
            if k not in dsems:
                dsems[k] = self._st.enter_context(nc.semaphore(f"d_{len(dsems)}"))
        with nc.Block() as block:
            def run(engname, eng):
                waited = self._waited[engname]
                for op in cur[engname]:
                    need = {}
                    for d in sorted(op.deps, key=lambda o: o.seq):
                        if d.dma is not None:
                            sem, val = dsems[d.dma], 16 * d.dman
                        else:
                            sem, val = esems[d.eng][d.idx // SEM_CHUNK], d.idx % SEM_CHUNK + 1
                        kk = id(sem)
                        if need.get(kk, (None, 0))[1] < val:
                            need[kk] = (sem, val)
                    for kk, (sem, val) in need.items():
                        if waited.get(kk, 0) >= val:
                            continue
                        waited[kk] = val
                        eng.wait_ge(sem, val)
                    inst = op.fn(eng)
                    if op.dma is not None:
                        inst.then_inc(dsems[op.dma], 16)
                    elif op.sig:
                        inst.then_inc(esems[engname][op.idx // SEM_CHUNK], 1)
                if engname == "sp" and final:
                    for k, n in self.dma_cnt.items():
                        eng.wait_ge(dsems[k], 16 * n)

            @block.tensor
            def _(eng):
                run("pe", eng)

            @block.scalar
            def _(eng):
                run("act", eng)

            @block.vector
            def _(eng):
                run("dve", eng)

            @block.gpsimd
            def _(eng):
                run("pool", eng)

            @block.sync
            def _(eng):
                run("sp", eng)
        for e in ENGS:
            self._start[e] = len(self.ops[e])
        self._prevbar = set(self.bar)
        if final:
            self._st.close()


D = 1024
DIN = 4624
T = 256
NE = 32
BLK = 256
RMS_EPS = 1e-6
LN_EPS = 1e-5
BIGS = 65536.0
NPE = 12
FDRIP = 2
DRIP = 1

CV = {}
_off = 0
for _n, _w in [("g_mix", 8), ("cw", 8 * 31), ("cb", 8), ("ln_g", 8), ("ln_b", 8), ("w4", 48), ("b4", 12),
               ("dt_bias", 1), ("a_log", 16), ("dsk", 16), ("ng", 8), ("g_xa", 8), ("g_mem", 8), ("g_moe", 8),
               ("rb", 36), ("flag", 1), ("thr", 16), ("iob", 64), ("pid2", 1)]:
    CV[_n] = (_off, _w)
    _off += _w
NCV = _off


def pk(v, k):
    return np.ascontiguousarray(np.asarray(v, np.float32).reshape(k, 128).T)


def build(nt, dbg=None, limit=None):
    NTOK = nt * T
    NCHK = NTOK // 128
    NSLOT_BLKS = (2 * NTOK + NE * (BLK - 1)) // BLK
    NSLOTS = NSLOT_BLKS * BLK
    nc = bass.Bass("TRN2", target_bir_lowering=False)
    dt_in = lambda name, shape, dt=F32: nc.dram_tensor(name, shape, dt, kind="ExternalInput").ap()
    xT_d = dt_in("xT", [D, 2 * NTOK])
    memT_d = dt_in("memT", [D, 256])
    cv_d = dt_in("cv", [128, NCV])
    cm_d = dt_in("cm", [128, 640])
    gbc_d = dt_in("gbc", [128, 2 * D])
    w_in_d = dt_in("w_in", [D, DIN])
    w_out_d = dt_in("w_out", [2 * D, D])
    w_q_d = dt_in("w_q", [D, D])
    w_k_d = dt_in("w_k", [D, D])
    w_v_d = dt_in("w_v", [D, D])
    w_o_d = dt_in("w_o", [D, D])
    w_r_d = dt_in("w_r", [D, 36])
    w_gate_d = dt_in("w_gate_r", [NE * 256, 2048])
    w_up_d = dt_in("w_up_r", [NE * 256, 2048])
    w_down_d = dt_in("w_down_r", [NE * 256, 2048])
    out_d = nc.dram_tensor("out", [NTOK, D], F32, kind="ExternalOutput").ap()
    CAT_d = nc.dram_tensor("CAT", [2 * D, NTOK], BF16, kind="Internal").ap()
    X2_d = nc.dram_tensor("X2", [NTOK, D], F32, kind="Internal").ap()
    H3_d = nc.dram_tensor("H3", [NTOK, D], BF16, kind="Internal").ap()
    Xs_d = nc.dram_tensor("Xs", [NSLOTS, D], BF16, kind="Internal").ap()
    Ys_d = nc.dram_tensor("Ys", [NSLOTS, D], F32, kind="Internal").ap()
    dbg_d = {}
    if dbg:
        for name, shape in dbg.items():
            dbg_d[name] = nc.dram_tensor("dbg_" + name, shape, F32, kind="ExternalOutput").ap()

    S = Sched(nc)
    S.limit = limit
    xT_v = xT_d.rearrange("(ko p) t -> p ko t", p=128)
    CAT_v = CAT_d.rearrange("(ko p) t -> p ko t", p=128)

    def bc3(ap2, n):
        return ap2.unsqueeze(2).to_broadcast((ap2.shape[0], ap2.shape[1], n))

    def bcm(ap2, n):
        return ap2.unsqueeze(1).to_broadcast((ap2.shape[0], n, ap2.shape[1]))

    with contextlib.ExitStack() as top:
        def sb(name, shape, dt, st=top):
            return st.enter_context(nc.sbuf_tensor(name + "_sb", shape, dt))

        def ps(name, shape, dt, st=top):
            return st.enter_context(nc.psum_tensor(name + "_ps", shape, dt))

        cv = sb("cv", [128, NCV], F32)
        cm = sb("cm", [128, 640], F32)
        cmb = sb("cmb", [128, 384], BF16)
        Abc = sb("Abc", [128, 16], F32)
        S.add("sp", lambda e: e.dma_start(out=cv[:], in_=cv_d), writes=["cv"], dma="c0")
        S.add("sp", lambda e: e.dma_start(out=cm[:], in_=cm_d), writes=["cm"], dma="c1")
        identf = cm[:, 0:128]
        onesf = cm[:, 128:256]
        tri_incl = cm[:, 256:384]
        stri_gt = cm[:, 384:512]
        S.add("dve", lambda e: e.tensor_copy(out=cmb[:, 0:256], in_=cm[:, 0:256]), reads=["cm"], writes=[("cmb", 0)])
        S.add("dve", lambda e: e.tensor_copy(out=cmb[:, 256:384], in_=cm[:, 512:640]), reads=["cm"], writes=[("cmb", 1)])
        ident = cmb[:, 0:128]
        ones = cmb[:, 128:256]
        stri_lt = cmb[:, 256:384]
        S.add("act", lambda e: e.activation(out=Abc[:], in_=cv[:, CV["a_log"][0]:CV["a_log"][0] + 16], func=AF.Exp), reads=["cv"], writes=["Abc"])
        S.add("dve", lambda e: e.tensor_scalar(out=Abc[:], in0=Abc[:], scalar1=-1.0, scalar2=None, op0=ALU.mult), reads=["Abc"], writes=["Abc"])

        def cvs(name, j=0, n=1):
            o = CV[name][0] + j
            return cv[:, o:o + n]

        epsr = sb("epsr", [128, 4], F32)
        S.add("dve", lambda e: e.memset(epsr[:, 0:1], RMS_EPS), writes=[("eps", 0)])
        S.add("dve", lambda e: e.memset(epsr[:, 1:2], LN_EPS), writes=[("eps", 1)])
        S.add("dve", lambda e: e.memset(epsr[:, 2:3], 1.0), writes=[("eps", 2)])
        ereg_top = top.enter_context(nc.gpsimd.register("ereg"))

        with contextlib.ExitStack() as pa:
            A_sb = lambda n, s, d: sb(n, s, d, pa)
            w_inT = A_sb("w_inT", [128, 8, DIN], BF16)
            win_v = w_in_d.rearrange("(ko p) n -> p ko n", p=128)
            for k in range(8):
                for (a, b) in [(0, 2048), (2048, 4096), (4096, DIN)]:
                    S.add("pool", lambda e, k=k, a=a, b=b: e.dma_start(out=w_inT[:, k, a:b], in_=win_v[:, k, a:b]),
                          writes=[("w_inT", (k, a))], dma="w_in")

            dg = A_sb("dg", [128, NPE * 8, 128], BF16)
            for k in range(NPE):
                for c in range(8):
                    S.add("dve", lambda e, k=k, c=c: e.tensor_scalar(out=dg[:, k * 8 + c, :], in0=ident, scalar1=cvs("cw", c * 31 + k), scalar2=None, op0=ALU.mult),
                          reads=["cmb", "cv"], writes=[("dg", k * 8 + c)])
            xt = A_sb("xt", [128, 8, T], F32)
            hT = A_sb("hT", [128, 8, T], BF16)
            sq = hT
            stt_ = [A_sb(f"stt{i}", [128, T], F32) for i in range(4)]
            rstd, mean_b, tmpa, tmpb = stt_
            sg = [A_sb(f"sg{i}", [128, T], BF16) for i in range(2)]
            upres = [A_sb(f"upre{i}", [128, 8, 30 + T], BF16) for i in range(2)]
            acc = A_sb("acc", [128, 8, T], F32)
            cat = A_sb("cat", [128, 16, T], BF16)
            szTs = [A_sb(f"szT{i}", [128, 8, T], BF16) for i in range(2)]
            xbp = A_sb("xbp", [128, 12, 3 + T], BF16)
            xbcTs = [A_sb(f"xbcT{i}", [128, 12, T], BF16) for i in range(2)]
            dgt = A_sb("dgt", [128, 8, 128], BF16)
            dcnt = [0]
            dtTs = [A_sb(f"dtT{i}", [16, T], F32) for i in range(2)]
            ygT = A_sb("ygT", [128, 8, T], BF16)
            tmp16 = A_sb("tmp16", [16, T], F32)
            xtok = A_sb("xtok", [128, 1024], BF16)
            xdt = A_sb("xdt", [128, 1024], BF16)
            xD = A_sb("xD", [128, 1024], BF16)
            Btok = A_sb("Btok", [128, 256], BF16)
            Et = A_sb("Et", [128, 8, 128], BF16)
            CBm = A_sb("CBm", [128, 2, 128], F32)
            mT = A_sb("mT", [128, 16, 128], BF16)
            sm = A_sb("sm", [128, 8, 16], F32)
            dttok, dtA, cs_sb, eacs, dif, wend, etot = [sm[:, i, :] for i in range(7)]
            ytok = A_sb("ytok", [128, 1024], BF16)
            t1 = A_sb("t1", [128, 1024], F32)
            state = A_sb("state", [128, 1024], F32)
            stateT = A_sb("stateT", [128, 1024], BF16)
            mm = [ps(f"mm{i}", [128, 512], F32, pa) for i in range(2)]
            stp = ps("stp", [128, 512], F32, pa)
            tp = ps("tp", [128, 1024], BF16, pa)
            yps = ps("yps", [128, 1024], F32, pa)
            big = ps("big", [128, 1024], F32, pa)
            xw = xD

            S.add("dve", lambda e: e.memset(state[:], 0.0), writes=["state"])
            S.add("pool", lambda e: e.memset(stateT[:], 0.0), writes=["stateT"])
            for u_ in range(2):
                S.add("pool", lambda e, u_=u_: e.memset(upres[u_][:], 0.0), writes=[f"upre{u_}"])
            S.add("pool", lambda e: e.memset(xbp[:], 0.0), writes=["xbp"])
            nmm = [0]

            def rms_h(src, gname, dst, n=T, skey='xt', dkey='hT'):
                S.add("act", lambda e: e.activation(out=sq[:, :, 0:n], in_=src[:, :, 0:n], func=AF.Square), reads=[skey], writes=["hT"])
                pb = mm[nmm[0] % 2]
                key = f"mm{nmm[0] % 2}"
                nmm[0] += 1
                for k in range(8):
                    S.add("pe", lambda e, k=k: e.matmul(pb[:, 0:n], lhsT=ones, rhs=sq[:, k, 0:n], start=(k == 0), stop=(k == 7)),
                          reads=["hT", "cmb"], writes=[key])
                S.add("act", lambda e: e.activation(out=tmpa[:, 0:n], in_=pb[:, 0:n], func=AF.Ln, scale=1.0 / D, bias=epsr[:, 0:1]), reads=[key, "eps"], writes=["tmpa"])
                S.add("act", lambda e: e.activation(out=rstd[:, 0:n], in_=tmpa[:, 0:n], func=AF.Exp, scale=-0.5), reads=["tmpa"], writes=["rstd"])
                for k in range(8):
                    S.add("dve", lambda e, k=k: e.scalar_tensor_tensor(out=dst[:, k, 0:n], in0=src[:, k, 0:n], scalar=cvs(gname, k), in1=rstd[:, 0:n],
                                                                         op0=ALU.mult, op1=ALU.mult),
                          reads=[skey, "cv", "rstd"], writes=[(dkey, k)])

            def diag_otf(wname, widx):
                slot = dcnt[0] % 8
                dcnt[0] += 1
                S.add("dve", lambda e, slot=slot: e.tensor_scalar(out=dgt[:, slot, :], in0=ident, scalar1=cvs(wname, widx), scalar2=None, op0=ALU.mult), reads=["cmb", "cv"], writes=[("dgt", slot)])
                return slot

            def inproj(j, ncols=128):
                pb = mm[nmm[0] % 2]
                key = f"mm{nmm[0] % 2}"
                nmm[0] += 1
                for k in range(8):
                    S.add("pe", lambda e, k=k, pb=pb: e.matmul(pb[0:ncols, 0:T], lhsT=w_inT[:, k, j * 128:j * 128 + ncols], rhs=hT[:, k, :],
                                                               start=(k == 0), stop=(k == 7)),
                          reads=["w_inT", "hT"], writes=[key])
                return pb, key

            def modeof(i):
                return "main" if i >= nt else ("pre_last" if i == nt - 1 else "pre")

            def frontA(i):
                mode = modeof(i)
                main = mode == "main"
                glu = mode in ("main", "pre_last")
                par = i % 2
                xbcT = xbcTs[par]
                xk = f"xbcT{par}"
                dtT = dtTs[par]
                dk = f"dtT{par}"
                upre, uk = upres[par], f"upre{par}"
                upo, uko = upres[1 - par], f"upre{1 - par}"
                szT, zk = szTs[par], f"szT{par}"
                S.add("sp", lambda e: e.dma_start(out=xt[:], in_=xT_v[:, :, i * T:(i + 1) * T]), writes=["xt"], dma="xt")
                rms_h(xt, "g_mix", hT)
                yield
                nxc = 12 if glu else 10
                for c in range(nxc):
                    pb, key = inproj(24 + c)
                    S.add("act", lambda e, pb=pb, c=c: e.activation(out=xbp[:, c, 3:3 + T], in_=pb[:, 0:T], func=AF.Copy), reads=[key], writes=[("xbp", c)])
                    pb2 = mm[nmm[0] % 2]
                    key2 = f"mm{nmm[0] % 2}"
                    nmm[0] += 1
                    for k in range(4):
                        slot = diag_otf("w4", c * 4 + k)
                        S.add("pe", lambda e, c=c, k=k, slot=slot, pb2=pb2: e.matmul(pb2[:, 0:T], lhsT=dgt[:, slot, :], rhs=xbp[:, c, k:k + T], start=(k == 0), stop=(k == 3)),
                              reads=[("xbp", c), ("dgt", slot)], writes=[key2])
                    S.add("act", lambda e, c=c, pb2=pb2: e.activation(out=xbcT[:, c, :], in_=pb2[:, 0:T], func=AF.Silu, bias=cvs("b4", c)), reads=[key2, "cv"], writes=[(xk, c)])
                    yield
                S.add("pool", lambda e: e.tensor_copy(out=xbp[:, :, 0:3], in_=xbp[:, :, T:T + 3]), reads=["xbp"], writes=["xbp"])
                pb, key = inproj(36, 16)
                S.add("act", lambda e, pb=pb: e.activation(out=tmp16[:], in_=pb[0:16, 0:T], func=AF.Exp, bias=cv[0:16, CV["dt_bias"][0]:CV["dt_bias"][0] + 1]),
                      reads=[key, "cv"], writes=["tmp16"])
                S.add("act", lambda e: e.activation(out=dtT[:], in_=tmp16[:], func=AF.Ln, bias=epsr[0:16, 2:3]), reads=["tmp16", "eps"], writes=[dk])
                yield
                if main:
                    for c in range(8):
                        pb, key = inproj(16 + c)
                        S.add("act", lambda e, pb=pb, c=c: e.activation(out=szT[:, c, :], in_=pb[:, 0:T], func=AF.Silu), reads=[key], writes=[(zk, c)])
                        yield
                if glu:
                    S.add("pool", lambda e: e.tensor_copy(out=upre[:, :, 0:30], in_=upo[:, :, T:T + 30]), reads=[uko], writes=[uk])
                    for c in range(8):
                        pb, key = inproj(8 + c)
                        s_ = sg[c % 2]
                        S.add("act", lambda e, pb=pb, s_=s_: e.activation(out=s_[:], in_=pb[:, 0:T], func=AF.Sigmoid), reads=[key], writes=[f"sg{c % 2}"])
                        pb, key = inproj(c)
                        S.add("dve", lambda e, pb=pb, s_=s_, c=c: e.tensor_tensor(out=upre[:, c, 30:30 + T], in0=pb[:, 0:T], in1=s_[:], op=ALU.mult),
                              reads=[key, f"sg{c % 2}"], writes=[(uk, c)])
                        yield

            def backA(i, fgen):
                mode = modeof(i)
                main = mode == "main"
                par = i % 2
                xbcT = xbcTs[par]
                xk = f"xbcT{par}"
                dtT = dtTs[par]
                dk = f"dtT{par}"
                upre, uk = upres[par], f"upre{par}"
                szT, zk = szTs[par], f"szT{par}"

                def conv31_gen():
                    for c in range(8):
                        pb = mm[nmm[0] % 2]
                        key = f"mm{nmm[0] % 2}"
                        nmm[0] += 1
                        for k in range(31):
                            if k < NPE:
                                S.add("pe", lambda e, c=c, k=k, pb=pb: e.matmul(pb[:, 0:T], lhsT=dg[:, k * 8 + c, :], rhs=upre[:, c, k:k + T], start=(k == 0), stop=(k == 30)),
                                      reads=[(uk, c), "dg"], writes=[key])
                            else:
                                slot = diag_otf("cw", c * 31 + k)
                                S.add("pe", lambda e, c=c, k=k, pb=pb, slot=slot: e.matmul(pb[:, 0:T], lhsT=dgt[:, slot, :], rhs=upre[:, c, k:k + T], start=(k == 0), stop=(k == 30)),
                                      reads=[(uk, c), ("dgt", slot)], writes=[key])
                        S.add("dve", lambda e, c=c, pb=pb: e.tensor_scalar(out=acc[:, c, :], in0=pb[:, 0:T], scalar1=cvs("cb", c), scalar2=None, op0=ALU.add),
                              reads=[key, "cv"], writes=[("acc", c)])
                        yield

                gen = conv31_gen() if main else iter(())

                def drip(n):
                    for _ in range(n):
                        if next(gen, "end") == "end":
                            break

                def fdrip(n):
                    for _ in range(n):
                        if next(fgen, "end") == "end":
                            break

                def dve(fn, reads, writes, n=DRIP):
                    drip(n)
                    if n:
                        fdrip(FDRIP)
                    S.add("dve", fn, reads=reads, writes=writes)

                def chunk(q):
                    cols = slice(q * 128, (q + 1) * 128)
                    for c in range(8):
                        S.add("pe", lambda e, c=c: e.transpose(out=tp[:, c * 128:(c + 1) * 128], in_=xbcT[:, c, cols], identity=ident), reads=[(xk, c), "cmb"], writes=["tp"])
                    S.add("act", lambda e: e.activation(out=xtok[:], in_=tp[:], func=AF.Copy), reads=["tp"], writes=["xtok"])
                    for g in range(2):
                        S.add("pe", lambda e, g=g: e.transpose(out=tp[:, g * 128:(g + 1) * 128], in_=xbcT[:, 8 + g, cols], identity=ident), reads=[(xk, 8 + g), "cmb"], writes=["tp"])
                    S.add("act", lambda e: e.activation(out=Btok[:], in_=tp[:, 0:256], func=AF.Copy), reads=["tp"], writes=["Btok"])
                    S.add("pe", lambda e: e.transpose(out=stp[:, 32:48], in_=dtT[0:16, cols], identity=identf[0:16, 0:16]), reads=[dk, "cm"], writes=["stp"])
                    dve(lambda e: e.tensor_copy(out=dttok, in_=stp[:, 32:48]), ["stp"], [("sm", 0)])
                    dve(lambda e: e.tensor_tensor(out=dtA, in0=dttok, in1=Abc[:], op=ALU.mult), [("sm", 0), "Abc"], [("sm", 1)], 0)
                    S.add("pe", lambda e: e.matmul(stp[:, 0:16], lhsT=tri_incl, rhs=dtA, start=True, stop=True), reads=[("sm", 1), "cm"], writes=["stp"])
                    S.add("pe", lambda e: e.matmul(stp[:, 16:32], lhsT=onesf, rhs=dtA, start=True, stop=True), reads=[("sm", 1), "cm"], writes=["stp"])
                    dve(lambda e: e.tensor_copy(out=cs_sb, in_=stp[:, 0:16]), ["stp"], [("sm", 2)])
                    S.add("act", lambda e: e.activation(out=eacs, in_=cs_sb, func=AF.Exp), reads=[("sm", 2)], writes=[("sm", 3)])
                    dve(lambda e: e.tensor_tensor(out=dif, in0=stp[:, 16:32], in1=cs_sb, op=ALU.subtract), ["stp", ("sm", 2)], [("sm", 4)], 0)
                    S.add("act", lambda e: e.activation(out=wend, in_=dif, func=AF.Exp), reads=[("sm", 4)], writes=[("sm", 5)])
                    S.add("act", lambda e: e.activation(out=etot, in_=stp[:, 16:32], func=AF.Exp), reads=["stp"], writes=[("sm", 6)])
                    dve(lambda e: e.tensor_tensor(out=wend, in0=wend, in1=dttok, op=ALU.mult), [("sm", 5), ("sm", 0)], [("sm", 5)])
                    xtok3 = xtok[:].rearrange("p (h d) -> p h d", d=64)
                    Rt = t1[:].rearrange("p (a b) -> p a b", b=128)
                    if main:
                        for g in range(2):
                            S.add("pe", lambda e, g=g: e.matmul(stp[:, 64 + g * 128:64 + (g + 1) * 128], lhsT=xbcT[:, 8 + g, cols], rhs=xbcT[:, 10 + g, cols], start=True, stop=True),
                                  reads=[(xk, 8 + g), (xk, 10 + g)], writes=["stp"])
                            dve(lambda e, g=g: e.tensor_tensor(out=CBm[:, g, :], in0=stp[:, 64 + g * 128:64 + (g + 1) * 128], in1=tri_incl, op=ALU.mult), ["stp", "cm"], [("CBm", g)])
                        S.add("pool", lambda e: e.tensor_tensor(out=xdt[:].rearrange("p (h d) -> p h d", d=64), in0=xtok3, in1=bc3(dttok, 64), op=ALU.mult),
                              reads=["xtok", ("sm", 0)], writes=["xdt"])
                        for half in range(2):
                            S.add("pool", lambda e, half=half: e.tensor_tensor(out=Rt, in0=bcm(tri_incl, 8), in1=bc3(dtA[:, half * 8:(half + 1) * 8], 128), op=ALU.mult),
                                  reads=["cm", ("sm", 1)], writes=["t1"])
                            for jj in range(2):
                                S.add("pe", lambda e, jj=jj: e.matmul(big[:, jj * 512:(jj + 1) * 512], lhsT=stri_gt, rhs=t1[:, jj * 512:(jj + 1) * 512],
                                                                      start=True, stop=True), reads=["t1", "cm"], writes=["big"])
                            S.add("act", lambda e: e.activation(out=Et[:].rearrange("p a b -> p (a b)"), in_=big[:], func=AF.Exp), reads=["big"], writes=["Et"])
                            dve(lambda e, half=half: e.tensor_tensor(out=mT[:, half * 8:(half + 1) * 8, :], in0=Et[:], in1=bcm(CBm[:, half, :], 8), op=ALU.mult),
                                ["Et", ("CBm", half)], [("mT", half)])
                        S.add("pool", lambda e: e.tensor_tensor(out=xD[:].rearrange("p (h d) -> p h d", d=64), in0=xtok3, in1=bc3(cvs("dsk", 0, 16), 64), op=ALU.mult),
                              reads=["xtok", "cv"], writes=["xD"])
                        for jj in range(2):
                            S.add("pe", lambda e, jj=jj: e.matmul(yps[:, jj * 512:(jj + 1) * 512], lhsT=ident, rhs=xD[:, jj * 512:(jj + 1) * 512], start=True, stop=False, skip_group_check=True),
                                  reads=["xD", "cmb"], writes=["yps"])
                        for h in range(16):
                            S.add("pe", lambda e, h=h: e.matmul(yps[:, h * 64:(h + 1) * 64], lhsT=mT[:, h, :], rhs=xdt[:, h * 64:(h + 1) * 64], start=False, stop=True, skip_group_check=True),
                                  reads=[("mT", h // 8), "xdt"], writes=["yps"])
                        for g in range(2):
                            S.add("pe", lambda e, g=g: e.matmul(big[:, g * 512:(g + 1) * 512], lhsT=xbcT[:, 10 + g, cols], rhs=stateT[:, g * 512:(g + 1) * 512], start=True, stop=True),
                                  reads=[(xk, 10 + g), "stateT"], writes=["big"])
                        dve(lambda e: e.tensor_tensor(out=t1[:].rearrange("p (h d) -> p h d", d=64), in0=big[:].rearrange("p (h d) -> p h d", d=64), in1=bc3(eacs, 64), op=ALU.mult),
                            ["big", ("sm", 3)], ["t1"])
                        dve(lambda e: e.tensor_tensor(out=ytok[:], in0=yps[:], in1=t1[:], op=ALU.add), ["yps", "t1"], ["ytok"])
                    S.add("pool", lambda e: e.tensor_tensor(out=xw[:].rearrange("p (h d) -> p h d", d=64), in0=xtok3, in1=bc3(wend, 64), op=ALU.mult), reads=["xtok", ("sm", 5)], writes=["xD"])
                    for g in range(2):
                        S.add("pe", lambda e, g=g: e.matmul(big[:, g * 512:(g + 1) * 512], lhsT=Btok[:, g * 128:(g + 1) * 128], rhs=xw[:, g * 512:(g + 1) * 512], start=True, stop=True),
                              reads=["Btok", "xD"], writes=["big"])
                    dve(lambda e: e.tensor_tensor(out=state[:].rearrange("p (h d) -> p h d", d=64), in0=state[:].rearrange("p (h d) -> p h d", d=64), in1=bc3(etot, 64), op=ALU.mult),
                        ["state", ("sm", 6)], ["state"])
                    dve(lambda e: e.tensor_tensor(out=state[:], in0=state[:], in1=big[:], op=ALU.add), ["state", "big"], ["state"])
                    S.add("act", lambda e: e.activation(out=stateT[:], in_=state[:], func=AF.Copy), reads=["state"], writes=["stateT"])
                    if main:
                        for c in range(8):
                            S.add("pe", lambda e, c=c: e.transpose(out=tp[:, c * 128:(c + 1) * 128], in_=ytok[:, c * 128:(c + 1) * 128], identity=ident), reads=["ytok", "cmb"], writes=["tp"])
                        dve(lambda e: e.tensor_tensor(out=ygT[:, :, cols], in0=tp[:].rearrange("p (c t) -> p c t", t=128), in1=szT[:, :, cols], op=ALU.mult),
                            ["tp", zk], ["ygT"])

                for q in range(T // 128):
                    chunk(q)
                fdrip(100000)
                if main:
                    drip(100000)
                    S.add("act", lambda e: e.activation(out=sq[:], in_=ygT[:], func=AF.Square), reads=["ygT"], writes=["hT"])
                    for g in range(2):
                        for cc in range(4):
                            S.add("pe", lambda e, g=g, cc=cc: e.matmul(stp[:, g * T:(g + 1) * T], lhsT=ones, rhs=sq[:, g * 4 + cc, :], start=(cc == 0), stop=(cc == 3)),
                                  reads=["hT", "cmb"], writes=["stp"])
                    rg = [rstd, mean_b]
                    rgk = ["rstd", "mean_b"]
                    for g in range(2):
                        S.add("act", lambda e, g=g: e.activation(out=tmpa[:], in_=stp[:, g * T:(g + 1) * T], func=AF.Ln, scale=1.0 / 512, bias=epsr[:, 0:1]), reads=["stp", "eps"], writes=["tmpa"])
                        S.add("act", lambda e, g=g: e.activation(out=rg[g][:], in_=tmpa[:], func=AF.Exp, scale=-0.5), reads=["tmpa"], writes=[rgk[g]])
                    for c in range(8):
                        S.add("dve", lambda e, c=c: e.scalar_tensor_tensor(out=cat[:, 8 + c, :], in0=ygT[:, c, :], scalar=cvs("ng", c), in1=rg[c // 4][:], op0=ALU.mult, op1=ALU.mult),
                              reads=["ygT", "cv", rgk[c // 4]], writes=[("cat", 8 + c)])
                    S.add("act", lambda e: e.activation(out=sq[:], in_=acc[:], func=AF.Copy), reads=["acc"], writes=["hT"])
                    for c in range(8):
                        S.add("pe", lambda e, c=c: e.matmul(stp[:, 0:T], lhsT=ones, rhs=sq[:, c, :], start=(c == 0), stop=(c == 7)), reads=["hT", "cmb"], writes=["stp"])
                    S.add("act", lambda e: e.activation(out=mean_b[:], in_=stp[:, 0:T], func=AF.Copy, scale=1.0 / D), reads=["stp"], writes=["mean_b"])
                    S.add("act", lambda e: e.activation(out=sq[:], in_=acc[:], func=AF.Square), reads=["acc"], writes=["hT"])
                    for c in range(8):
                        S.add("pe", lambda e, c=c: e.matmul(stp[:, T:2 * T], lhsT=ones, rhs=sq[:, c, :], start=(c == 0), stop=(c == 7)), reads=["hT", "cmb"], writes=["stp"])
                    S.add("dve", lambda e: e.tensor_tensor(out=tmpa[:], in0=mean_b[:], in1=mean_b[:], op=ALU.mult), reads=["mean_b"], writes=["tmpa"])
                    S.add("dve", lambda e: e.scalar_tensor_tensor(out=tmpb[:], in0=stp[:, T:2 * T], scalar=1.0 / D, in1=tmpa[:], op0=ALU.mult, op1=ALU.subtract),
                          reads=["stp", "tmpa"], writes=["tmpb"])
                    S.add("act", lambda e: e.activation(out=tmpa[:], in_=tmpb[:], func=AF.Ln, bias=epsr[:, 1:2]), reads=["tmpb", "eps"], writes=["tmpa"])
                    S.add("act", lambda e: e.activation(out=rstd[:], in_=tmpa[:], func=AF.Exp, scale=-0.5), reads=["tmpa"], writes=["rstd"])
                    for c in range(8):
                        S.add("dve", lambda e, c=c: e.tensor_tensor(out=acc[:, c, :], in0=acc[:, c, :], in1=mean_b[:], op=ALU.subtract), reads=[("acc", c), "mean_b"], writes=[("acc", c)])
                        S.add("dve", lambda e, c=c: e.scalar_tensor_tensor(out=acc[:, c, :], in0=acc[:, c, :], scalar=cvs("ln_g", c), in1=rstd[:], op0=ALU.mult, op1=ALU.mult),
                              reads=[("acc", c), "rstd", "cv"], writes=[("acc", c)])
                        S.add("act", lambda e, c=c: e.activation(out=cat[:, c, :], in_=acc[:, c, :], func=AF.Silu, bias=cvs("ln_b", c)), reads=[("acc", c), "cv"], writes=[("cat", c)])
                    S.add("sp", lambda e: e.dma_start(out=CAT_v[:, :, (i - nt) * T:(i - nt + 1) * T], in_=cat[:]), reads=["cat"], writes=[("CAT", i - nt)], dma="catst")

            for _ in frontA(0):
                pass
            for i in range(2 * nt):
                fgen = frontA(i + 1) if i + 1 < 2 * nt else iter(())
                backA(i, fgen)
                if i == nt - 1:
                    S.add("dve", lambda e: e.tensor_scalar(out=state[:], in0=state[:], scalar1=cvs("flag"), scalar2=None, op0=ALU.mult), reads=["state", "cv"], writes=["state"])
                    S.add("act", lambda e: e.activation(out=stateT[:], in_=state[:], func=AF.Copy), reads=["state"], writes=["stateT"])
            S.barrier()
            S.emit_phase()


        Aall = sb("Aall", [128, NCHK, NE], F32)
        Wall = sb("Wall", [128, NCHK, NE], F32)
        Pall = sb("Pall", [128, NCHK, NE], F32)
        cnt = sb("cnt", [128, NE], F32)
        S.add("dve", lambda e: e.memset(cnt[:], 0.0), writes=["cnt"])
        build_bc(nc, S, nt, sb, ps, cv, cvs, cm, cmb, epsr, Aall, Wall, Pall, cnt, bc3, bcm,
                 dict(CAT_v=CAT_v, xT_v=xT_v, w_out_d=w_out_d, memT_d=memT_d, w_q_d=w_q_d, w_k_d=w_k_d, w_v_d=w_v_d, w_o_d=w_o_d, w_r_d=w_r_d, gbc_d=gbc_d,
                      X2_d=X2_d, H3_d=H3_d, Xs_d=Xs_d, Ys_d=Ys_d, out_d=out_d, w_gate_d=w_gate_d, w_up_d=w_up_d, w_down_d=w_down_d,
                      NSLOT_BLKS=NSLOT_BLKS, NCHK=NCHK, ereg=ereg_top), dbg, dbg_d)
    return nc


def build_bc(nc, S, nt, sb, ps, cv, cvs, cm, cmb, epsr, Aall, Wall, Pall, cnt, bc3, bcm, dd, dbg, dbg_d):
    CAT_v = dd["CAT_v"]
    xT_v = dd["xT_v"]
    NCHK = dd["NCHK"]
    NB = dd["NSLOT_BLKS"]
    identf = cm[:, 0:128]
    ident = cmb[:, 0:128]
    ones = cmb[:, 128:256]
    stri_lt = cmb[:, 256:384]
    with contextlib.ExitStack() as pb_:
        B = lambda n, s_, d: sb(n, s_, d, pb_)
        wq = B("wq", [128, 8, D], BF16)
        w_outT = B("w_outT", [128, 16, D], BF16)
        catb = B("catb", [128, 16, T], BF16)
        wo = B("wo", [128, 8, D], BF16)
        wkv = B("wkv", [128, 8, D], BF16)
        wv = B("wv", [128, 8, D], BF16)
        kT = B("kT", [128, 8, 256], BF16)
        vtok = B("vtok", [128, 2, D], BF16)
        wr = B("wr", [128, 8, 36], F32)
        gmo = B("gmo", [128, D], F32)
        memT = B("memTs", [128, 8, 256], F32)
        mnT = B("mnT", [128, 8, 256], BF16)
        xts = [B(f"xtb{i}", [128, 8, T], F32) for i in range(2)]
        tmpa2 = B("tmpa2", [128, T], F32)
        sq = B("sqb", [128, 8, T], BF16)
        hT = B("hTb", [128, 8, T], BF16)
        tmpa = B("tmpab", [128, T], F32)
        rstd = B("rstdb", [128, T], F32)
        rinv = B("rinv", [128, T], F32)
        qTs = [B(f"qT{i}", [128, 8, T], BF16) for i in range(2)]
        pT = B("pT", [128, 2, T], BF16)
        oT = B("oT", [128, 8, T], BF16)
        x2tok = B("x2tok", [128, D], F32)
        junk = B("junk", [128, D], F32)
        h3tok = B("h3tok", [128, D], BF16)
        r1 = B("r1", [128, 16], F32)
        lg = B("lg", [128, 36], F32)
        lm = B("lm", [128, 32], F32)
        ex = B("ex", [128, 32], F32)
        gm = B("gm", [128, 8], F32)
        m8 = B("m8", [128, 8], F32)
        Abf = B("Abf", [128, 32], BF16)
        mm = [ps(f"mmb{i}", [128, 512], F32, pb_) for i in range(2)]
        stp = ps("stpb", [128, 512], F32, pb_)
        tp2 = ps("tp2", [128, 1024], F32, pb_)
        nmm = [0]

        def nextmm():
            pbk = mm[nmm[0] % 2]
            key = f"mmb{nmm[0] % 2}"
            nmm[0] += 1
            return pbk, key

        def rms_h(src, skey, gname, dst, dkey, n):
            S.add("act", lambda e: e.activation(out=sq[:, :, 0:n], in_=src[:, :, 0:n], func=AF.Square), reads=[skey], writes=["sqb"])
            pbk, key = nextmm()
            for k in range(8):
                S.add("pe", lambda e, k=k: e.matmul(pbk[:, 0:n], lhsT=ones, rhs=sq[:, k, 0:n], start=(k == 0), stop=(k == 7)), reads=["sqb", "cmb"], writes=[key])
            S.add("act", lambda e: e.activation(out=tmpa[:, 0:n], in_=pbk[:, 0:n], func=AF.Ln, scale=1.0 / D, bias=epsr[:, 0:1]), reads=[key, "eps"], writes=["tmpab"])
            S.add("act", lambda e: e.activation(out=rstd[:, 0:n], in_=tmpa[:, 0:n], func=AF.Exp, scale=-0.5), reads=["tmpab"], writes=["rstdb"])
            for k in range(8):
                S.add("dve", lambda e, k=k: e.scalar_tensor_tensor(out=dst[:, k, 0:n], in0=src[:, k, 0:n], scalar=cvs(gname, k), in1=rstd[:, 0:n], op0=ALU.mult, op1=ALU.mult),
                      reads=[skey, "cv", "rstdb"], writes=[(dkey, k)])

        def wload(dst, key, src_d):
            v = src_d.rearrange("(ko p) n -> p ko n", p=128)
            for k in range(8):
                S.add("pool", lambda e, k=k: e.dma_start(out=dst[:, k, :], in_=v[:, k, :]), writes=[(key, k)], dma=key)

        zt = B("zt", [128, D], BF16)
        S.add("pool", lambda e: e.memset(zt[:], 0.0), writes=["zt"])
        NR = dd["NSLOT_BLKS"] * BLK // 128
        xs_v = dd["Xs_d"].rearrange("(r p) d -> p r d", p=128)
        for zr0 in range(0, NR, 16):
            zr1 = min(NR, zr0 + 16)
            S.add("sp", lambda e, zr0=zr0, zr1=zr1: e.dma_start(out=xs_v[:, zr0:zr1, :], in_=zt[:].unsqueeze(1).to_broadcast((128, zr1 - zr0, D))), reads=["zt"], writes=["Xs"], dma="xsz")
        wout_v = dd["w_out_d"].rearrange("(ko p) n -> p ko n", p=128)
        for k in range(16):
            S.add("pool", lambda e, k=k: e.dma_start(out=w_outT[:, k, :], in_=wout_v[:, k, :]), writes=[("w_outT", k)], dma="w_out")
        wload(wq, "wq", dd["w_q_d"])
        wload(wo, "wo", dd["w_o_d"])
        wload(wkv, "wkv", dd["w_k_d"])
        S.add("sp", lambda e: e.dma_start(out=memT[:], in_=dd["memT_d"].rearrange("(ko p) t -> p ko t", p=128)), writes=["memTs"], dma="memT")
        S.add("sp", lambda e: e.dma_start(out=wr[:], in_=dd["w_r_d"].rearrange("(ko p) n -> p ko n", p=128)), writes=["wr"], dma="wr")
        S.add("sp", lambda e: e.dma_start(out=gmo[:], in_=dd["gbc_d"][:, 0:D]), writes=["gmo"], dma="gmo")
        for k in range(8):
            S.add("dve", lambda e, k=k: e.tensor_scalar(out=wr[:, k, :], in0=wr[:, k, :], scalar1=cvs("g_moe", k), scalar2=None, op0=ALU.mult), reads=["wr", "cv"], writes=["wr"])
        rms_h(memT, "memTs", "g_mem", mnT, "mnT", 256)
        for j in range(8):
            pbk, key = nextmm()
            for k in range(8):
                S.add("pe", lambda e, k=k, j=j, pbk=pbk: e.matmul(pbk[:, 0:256], lhsT=wkv[:, k, j * 128:(j + 1) * 128], rhs=mnT[:, k, :], start=(k == 0), stop=(k == 7)),
                      reads=["wkv", "mnT"], writes=[key])
            S.add("act", lambda e, j=j, pbk=pbk: e.activation(out=kT[:, j, :], in_=pbk[:, 0:256], func=AF.Copy), reads=[key], writes=[("kT", j)])
        wload(wv, "wv", dd["w_v_d"])
        for jb in range(2):
            for dh in range(2):
                pbk, key = nextmm()
                for k in range(8):
                    S.add("pe", lambda e, k=k, jb=jb, dh=dh, pbk=pbk: e.matmul(pbk[:, 0:512], lhsT=mnT[:, k, jb * 128:(jb + 1) * 128], rhs=wv[:, k, dh * 512:(dh + 1) * 512], start=(k == 0), stop=(k == 7)),
                          reads=["wv", "mnT"], writes=[key])
                S.add("act", lambda e, jb=jb, dh=dh, pbk=pbk: e.activation(out=vtok[:, jb, dh * 512:(dh + 1) * 512], in_=pbk[:, 0:512], func=AF.Copy), reads=[key], writes=[("vtok", jb * 2 + dh)])

        def frontB(i):
            par = i % 2
            xt, xk = xts[par], f"xtb{par}"
            qT, qk = qTs[par], f"qT{par}"
            S.add("sp", lambda e: e.dma_start(out=xt[:], in_=xT_v[:, :, (nt + i) * T:(nt + i + 1) * T]), writes=[xk], dma=f"xtb{par}")
            S.add("sp", lambda e: e.dma_start(out=catb[:], in_=CAT_v[:, :, i * T:(i + 1) * T]), reads=[("CAT", i)], writes=["catb"], dma="catb")
            for dz in range(8):
                pbk, key = nextmm()
                for k in range(16):
                    S.add("pe", lambda e, k=k, dz=dz, pbk=pbk: e.matmul(pbk[:, 0:T], lhsT=w_outT[:, k, dz * 128:(dz + 1) * 128], rhs=catb[:, k, :], start=(k == 0), stop=(k == 15)),
                          reads=["w_outT", "catb"], writes=[key])
                S.add("dve", lambda e, dz=dz, pbk=pbk: e.tensor_tensor(out=xt[:, dz, :], in0=xt[:, dz, :], in1=pbk[:, 0:T], op=ALU.add), reads=[xk, key], writes=[xk])
                yield
            rms_h(xt, xk, "g_xa", hT, "hTb", T)
            yield
            for j in range(8):
                pbk, key = nextmm()
                for k in range(8):
                    S.add("pe", lambda e, k=k, j=j, pbk=pbk: e.matmul(pbk[:, 0:T], lhsT=wq[:, k, j * 128:(j + 1) * 128], rhs=hT[:, k, :], start=(k == 0), stop=(k == 7)), reads=["wq", "hTb"], writes=[key])
                S.add("act", lambda e, j=j, pbk=pbk: e.activation(out=qT[:, j, :], in_=pbk[:, 0:T], func=AF.Copy), reads=[key], writes=[(qk, j)])
                yield

        def backB(i, fgen):
            par = i % 2
            xt, xk = xts[par], f"xtb{par}"
            qT, qk = qTs[par], f"qT{par}"

            def fdrip(n):
                for _ in range(n):
                    if next(fgen, "end") == "end":
                        break

            for h in range(4):
                fdrip(2)
                for jb in range(2):
                    pbk, key = nextmm()
                    for dc in range(2):
                        S.add("pe", lambda e, h=h, jb=jb, dc=dc, pbk=pbk: e.matmul(pbk[:, 0:T], lhsT=kT[:, h * 2 + dc, jb * 128:(jb + 1) * 128], rhs=qT[:, h * 2 + dc, :], start=(dc == 0), stop=(dc == 1)),
                              reads=["kT", (qk, h * 2 + dc)], writes=[key])
                    S.add("act", lambda e, jb=jb, pbk=pbk: e.activation(out=pT[:, jb, :], in_=pbk[:, 0:T], func=AF.Exp, scale=1.0 / 16.0), reads=[key], writes=[("pT", jb)])
                for jb in range(2):
                    S.add("pe", lambda e, jb=jb: e.matmul(stp[:, 0:T], lhsT=ones, rhs=pT[:, jb, :], start=(jb == 0), stop=(jb == 1)), reads=[("pT", jb), "cmb"], writes=["stpb"])
                S.add("act", lambda e: e.activation(out=tmpa2[:], in_=stp[:, 0:T], func=AF.Ln), reads=["stpb"], writes=["tmpa2"])
                S.add("act", lambda e: e.activation(out=rinv[:], in_=tmpa2[:], func=AF.Exp, scale=-1.0), reads=["tmpa2"], writes=["rinv"])
                for dvc in range(2):
                    pbk, key = nextmm()
                    for jb in range(2):
                        S.add("pe", lambda e, h=h, jb=jb, dvc=dvc, pbk=pbk: e.matmul(pbk[:, 0:T], lhsT=vtok[:, jb, h * 256 + dvc * 128:h * 256 + (dvc + 1) * 128], rhs=pT[:, jb, :], start=(jb == 0), stop=(jb == 1)),
                              reads=["vtok", ("pT", jb)], writes=[key])
                    S.add("dve", lambda e, h=h, dvc=dvc, pbk=pbk: e.tensor_tensor(out=oT[:, h * 2 + dvc, :], in0=pbk[:, 0:T], in1=rinv[:], op=ALU.mult), reads=[key, "rinv"], writes=[("oT", h * 2 + dvc)])
            for dz in range(8):
                fdrip(1)
                pbk, key = nextmm()
                for k in range(8):
                    S.add("pe", lambda e, k=k, dz=dz, pbk=pbk: e.matmul(pbk[:, 0:T], lhsT=wo[:, k, dz * 128:(dz + 1) * 128], rhs=oT[:, k, :], start=(k == 0), stop=(k == 7)), reads=["wo", "oT"], writes=[key])
                S.add("dve", lambda e, dz=dz, pbk=pbk: e.tensor_tensor(out=xt[:, dz, :], in0=xt[:, dz, :], in1=pbk[:, 0:T], op=ALU.add), reads=[xk, key], writes=[xk])

            def chunkB(q):
                fdrip(1)
                ci = i * (T // 128) + q
                cols = slice(q * 128, (q + 1) * 128)
                rows = slice(ci * 128, (ci + 1) * 128)
                for c in range(8):
                    S.add("pe", lambda e, c=c: e.transpose(out=tp2[:, c * 128:(c + 1) * 128], in_=xt[:, c, cols], identity=identf), reads=[xk, "cm"], writes=["tp2"])
                S.add("act", lambda e: e.activation(out=x2tok[:], in_=tp2[:], func=AF.Copy), reads=["tp2"], writes=["x2tok"])
                S.add("sp", lambda e: e.dma_start(out=dd["X2_d"][rows, :], in_=x2tok[:]), reads=["x2tok"], writes=[("X2", ci)], dma="x2st")
                S.add("act", lambda e: e.activation(out=junk[:], in_=x2tok[:], func=AF.Square, accum_out=r1[:, 0:1]), reads=["x2tok"], writes=["junk", ("r1", 0)])
                S.add("act", lambda e: e.activation(out=r1[:, 1:2], in_=r1[:, 0:1], func=AF.Ln, scale=1.0 / D, bias=epsr[:, 0:1]), reads=[("r1", 0), "eps"], writes=[("r1", 1)])
                S.add("act", lambda e: e.activation(out=r1[:, 2:3], in_=r1[:, 1:2], func=AF.Exp, scale=-0.5), reads=[("r1", 1)], writes=[("r1", 2)])
                rs3 = r1[:, 2:3]
                S.add("dve", lambda e: e.scalar_tensor_tensor(out=h3tok[:], in0=x2tok[:], scalar=rs3, in1=gmo[:], op0=ALU.mult, op1=ALU.mult), reads=["x2tok", ("r1", 2), "gmo"], writes=["h3tok"])
                S.add("sp", lambda e: e.dma_start(out=dd["H3_d"][rows, :], in_=h3tok[:]), reads=["h3tok"], writes=[("H3", ci)], dma="h3st")
                for k in range(8):
                    S.add("pe", lambda e, k=k: e.matmul(stp[:, 0:36], lhsT=xt[:, k, cols], rhs=wr[:, k, :], start=(k == 0), stop=(k == 7)), reads=[xk, "wr"], writes=["stpb"])
                S.add("dve", lambda e: e.scalar_tensor_tensor(out=lg[:], in0=stp[:, 0:36], scalar=rs3, in1=cvs("rb", 0, 36), op0=ALU.mult, op1=ALU.add), reads=["stpb", ("r1", 2), "cv"], writes=["lg"])
                S.add("dve", lambda e: e.tensor_reduce(out=r1[:, 3:4], in_=lg[:, 0:4], axis=AX.X, op=ALU.max), reads=["lg"], writes=[("r1", 3)])
                S.add("dve", lambda e: e.tensor_scalar(out=r1[:, 4:5], in0=r1[:, 3:4], scalar1=-1.0, scalar2=None, op0=ALU.mult), reads=[("r1", 3)], writes=[("r1", 4)])
                S.add("act", lambda e: e.activation(out=gm[:, 0:4], in_=lg[:, 0:4], func=AF.Exp, bias=r1[:, 4:5], accum_out=r1[:, 5:6]), reads=["lg", ("r1", 4)], writes=[("gm", 0), ("r1", 5)])
                S.add("dve", lambda e: e.reciprocal(out=r1[:, 6:7], in_=r1[:, 5:6]), reads=[("r1", 5)], writes=[("r1", 6)])
                S.add("dve", lambda e: e.tensor_scalar(out=gm[:, 4:8], in0=lg[:, 0:4], scalar1=r1[:, 3:4], scalar2=None, op0=ALU.is_equal), reads=["lg", ("r1", 3)], writes=[("gm", 1)])
                S.add("dve", lambda e: e.tensor_scalar(out=gm[:, 4:8], in0=gm[:, 4:8], scalar1=1.0e4, scalar2=-1.0e4, op0=ALU.mult, op1=ALU.add), reads=[("gm", 1)], writes=[("gm", 1)])
                S.add("dve", lambda e: e.tensor_tensor(out=lm[:].rearrange("p (g e) -> p g e", e=8), in0=lg[:, 4:36].rearrange("p (g e) -> p g e", e=8), in1=bc3(gm[:, 4:8], 8), op=ALU.add),
                      reads=["lg", ("gm", 1)], writes=["lm"])
                S.add("dve", lambda e: e.max(out=m8[:], in_=lm[:]), reads=["lm"], writes=["m8"])
                S.add("dve", lambda e: e.tensor_scalar(out=r1[:, 7:8], in0=m8[:, 0:1], scalar1=-1.0, scalar2=None, op0=ALU.mult), reads=["m8"], writes=[("r1", 7)])
                S.add("dve", lambda e: e.tensor_scalar(out=Aall[:, ci, :], in0=lm[:], scalar1=m8[:, 1:2], scalar2=None, op0=ALU.is_ge), reads=["lm", "m8"], writes=[("Aall", ci)])
                S.add("act", lambda e: e.activation(out=ex[:], in_=lm[:], func=AF.Exp, bias=r1[:, 7:8]), reads=["lm", ("r1", 7)], writes=["ex"])
                S.add("dve", lambda e: e.tensor_tensor(out=ex[:], in0=ex[:], in1=Aall[:, ci, :], op=ALU.mult), reads=["ex", ("Aall", ci)], writes=["ex"])
                S.add("dve", lambda e: e.tensor_reduce(out=r1[:, 8:9], in_=ex[:], axis=AX.X, op=ALU.add), reads=["ex"], writes=[("r1", 8)])
                S.add("dve", lambda e: e.reciprocal(out=r1[:, 9:10], in_=r1[:, 8:9]), reads=[("r1", 8)], writes=[("r1", 9)])
                S.add("dve", lambda e: e.tensor_tensor(out=r1[:, 10:11], in0=r1[:, 9:10], in1=r1[:, 6:7], op=ALU.mult), reads=[("r1", 9), ("r1", 6)], writes=[("r1", 10)])
                S.add("dve", lambda e: e.tensor_scalar(out=Wall[:, ci, :], in0=ex[:], scalar1=r1[:, 10:11], scalar2=None, op0=ALU.mult), reads=["ex", ("r1", 10)], writes=[("Wall", ci)])
                S.add("pool", lambda e: e.tensor_copy(out=Abf[:], in_=Aall[:, ci, :]), reads=[("Aall", ci)], writes=["Abf"])
                S.add("pe", lambda e: e.matmul(stp[:, 64:96], lhsT=stri_lt, rhs=Abf[:], start=True, stop=True), reads=["Abf", "cmb"], writes=["stpb"])
                S.add("pe", lambda e: e.matmul(stp[:, 96:128], lhsT=ones, rhs=Abf[:], start=True, stop=True), reads=["Abf", "cmb"], writes=["stpb"])
                S.add("dve", lambda e: e.tensor_tensor(out=Pall[:, ci, :], in0=stp[:, 64:96], in1=cnt[:], op=ALU.add), reads=["stpb", "cnt"], writes=[("Pall", ci)])
                S.add("dve", lambda e: e.tensor_tensor(out=cnt[:], in0=cnt[:], in1=stp[:, 96:128], op=ALU.add), reads=["stpb", "cnt"], writes=["cnt"])

            for q in range(T // 128):
                chunkB(q)
            fdrip(100000)

        for _ in frontB(0):
            pass
        for i in range(nt):
            backB(i, frontB(i + 1) if i + 1 < nt else iter(()))
        S.barrier()
        S.emit_phase()

    with contextlib.ExitStack() as pc_:
        C = lambda n, s_, d: sb(n, s_, d, pc_)
        gfin = C("gfin", [128, D], F32)
        S.add("sp", lambda e: e.dma_start(out=gfin[:], in_=dd["gbc_d"][:, D:2 * D]), writes=["gfin"], dma="gfin")
        nblk = C("nblk", [128, NE], F32)
        cA = C("cA", [128, NE], F32)
        cB = C("cB", [128, NE], F32)
        sslot = C("sslot", [128, NE], F32)
        shi = C("shi", [128, NCHK], F32)
        slo = C("slo", [128, NCHK], F32)
        whi = C("whi", [128, NCHK], F32)
        wlo = C("wlo", [128, NCHK], F32)
        shi_i = C("shi_i", [128, NCHK], I32)
        slo_i = C("slo_i", [128, NCHK], I32)
        eb = C("eb", [128, NB], F32)
        ebi = C("ebi", [128, NB], I32)
        widf = C("widf", [128, NB], F32)
        widx0 = C("widx0", [128, NB], I32)
        rr = C("rr", [128, 2, 4], F32)
        c1 = contextlib.ExitStack()
        C1 = lambda n, s_, d: sb(n, s_, d, c1)
        cmp = C1("cmp", [128, NE, 16], F32)
        t1 = C1("t1c", [128, NCHK, NE], F32)
        sd = C1("sd", [128, NCHK, NE], F32)
        t2 = C1("t2c", [128, NCHK, NE], F32)
        cmpb = C1("cmpb", [128, NB, NE], F32)
        S.add("dve", lambda e: e.tensor_tensor(out=cmp[:], in0=bc3(cnt[:], 16), in1=bcm(cvs("thr", 0, 16), NE), op=ALU.is_gt), reads=["cnt", "cv"], writes=["cmp"])
        S.add("dve", lambda e: e.tensor_reduce(out=nblk[:], in_=cmp[:], axis=AX.X, op=ALU.add), reads=["cmp"], writes=["nblk"])
        S.add("dve", lambda e: e.tensor_copy(out=cA[:], in_=nblk[:]), reads=["nblk"], writes=["cA"])
        cur, oth, ck, ok_ = cA, cB, "cA", "cB"
        for sh in (1, 2, 4, 8, 16):
            S.add("dve", lambda e, cur=cur, oth=oth, sh=sh: e.tensor_copy(out=oth[:, 0:sh], in_=cur[:, 0:sh]), reads=[ck], writes=[ok_])
            S.add("dve", lambda e, cur=cur, oth=oth, sh=sh: e.tensor_tensor(out=oth[:, sh:NE], in0=cur[:, sh:NE], in1=cur[:, 0:NE - sh], op=ALU.add), reads=[ck], writes=[ok_])
            cur, oth, ck, ok_ = oth, cur, ok_, ck
        endb, endk = cur, ck
        S.add("dve", lambda e: e.tensor_tensor(out=sslot[:], in0=endb[:], in1=nblk[:], op=ALU.subtract), reads=[endk, "nblk"], writes=["sslot"])
        S.add("dve", lambda e: e.tensor_scalar(out=sslot[:], in0=sslot[:], scalar1=float(BLK), scalar2=None, op0=ALU.mult), reads=["sslot"], writes=["sslot"])
        S.add("dve", lambda e: e.tensor_tensor(out=t1[:], in0=Pall[:], in1=bcm(sslot[:], NCHK), op=ALU.add), reads=["Pall", "sslot"], writes=["t1c"])
        S.add("dve", lambda e: e.scalar_tensor_tensor(out=sd[:].rearrange("p c e -> p (c e)"), in0=t1[:].rearrange("p c e -> p (c e)"), scalar=1.0, in1=Aall[:].rearrange("p c e -> p (c e)"), op0=ALU.add, op1=ALU.mult),
              reads=["t1c", "Aall"], writes=["sd"])
        S.add("dve", lambda e: e.tensor_scalar(out=sd[:], in0=sd[:], scalar1=-1.0, scalar2=None, op0=ALU.add), reads=["sd"], writes=["sd"])
        S.add("dve", lambda e: e.tensor_reduce(out=shi[:], in_=sd[:], axis=AX.X, op=ALU.max), reads=["sd"], writes=["shi"])
        S.add("dve", lambda e: e.tensor_scalar(out=t2[:], in0=t1[:], scalar1=-1.0, scalar2=BIGS, op0=ALU.mult, op1=ALU.add), reads=["t1c"], writes=["t2c"])
        S.add("dve", lambda e: e.tensor_tensor(out=t2[:], in0=t2[:], in1=Aall[:], op=ALU.mult), reads=["t2c", "Aall"], writes=["t2c"])
        S.add("dve", lambda e: e.tensor_reduce(out=slo[:], in_=t2[:], axis=AX.X, op=ALU.max), reads=["t2c"], writes=["slo"])
        S.add("dve", lambda e: e.tensor_scalar(out=slo[:], in0=slo[:], scalar1=-1.0, scalar2=BIGS, op0=ALU.mult, op1=ALU.add), reads=["slo"], writes=["slo"])
        S.add("dve", lambda e: e.tensor_tensor(out=t2[:], in0=sd[:], in1=bc3(shi[:], NE), op=ALU.is_equal), reads=["sd", "shi"], writes=["t2c"])
        S.add("dve", lambda e: e.tensor_tensor(out=t2[:], in0=t2[:], in1=Wall[:], op=ALU.mult), reads=["t2c", "Wall"], writes=["t2c"])
        S.add("dve", lambda e: e.tensor_reduce(out=whi[:], in_=t2[:], axis=AX.X, op=ALU.add), reads=["t2c"], writes=["whi"])
        S.add("dve", lambda e: e.tensor_reduce(out=wlo[:], in_=Wall[:], axis=AX.X, op=ALU.add), reads=["Wall"], writes=["wlo"])
        S.add("dve", lambda e: e.tensor_tensor(out=wlo[:], in0=wlo[:], in1=whi[:], op=ALU.subtract), reads=["wlo", "whi"], writes=["wlo"])
        S.add("dve", lambda e: e.tensor_copy(out=shi_i[:], in_=shi[:]), reads=["shi"], writes=["shi_i"])
        S.add("dve", lambda e: e.tensor_copy(out=slo_i[:], in_=slo[:]), reads=["slo"], writes=["slo_i"])
        S.add("dve", lambda e: e.tensor_tensor(out=cmpb[:], in0=bcm(endb[:], NB), in1=bc3(cvs("iob", 0, NB), NE), op=ALU.is_le), reads=[endk, "cv"], writes=["cmpb"])
        S.add("dve", lambda e: e.tensor_reduce(out=eb[:], in_=cmpb[:], axis=AX.X, op=ALU.add), reads=["cmpb"], writes=["eb"])
        S.add("dve", lambda e: e.tensor_scalar(out=widf[:], in0=eb[:], scalar1=256.0, scalar2=cvs("pid2"), op0=ALU.mult, op1=ALU.add), reads=["eb", "cv"], writes=["widf"])
        S.add("dve", lambda e: e.tensor_scalar(out=widf[:], in0=widf[:], scalar1=0.5, scalar2=None, op0=ALU.mult), reads=["widf"], writes=["widf"])
        S.add("dve", lambda e: e.tensor_copy(out=widx0[:], in_=widf[:]), reads=["widf"], writes=["widx"])

        c1.close()
        S.barrier()
        c2 = contextlib.ExitStack()
        C = lambda n, s_, d: sb(n, s_, d, c2)
        hbuf = [C(f"hbuf{i}", [128, D], BF16) for i in range(2)]
        for ci in range(NCHK):
            hb, hk = hbuf[ci % 2], f"hbuf{ci % 2}"
            S.add("sp", lambda e, ci=ci, hb=hb: e.dma_start(out=hb[:], in_=dd["H3_d"][ci * 128:(ci + 1) * 128, :]), reads=[("H3", ci)], writes=[hk], dma=f"hl{ci % 2}")
            for kk, idx in enumerate((shi_i, slo_i)):
                S.add("pool", lambda e, ci=ci, hb=hb, idx=idx: e.indirect_dma_start(out=dd["Xs_d"], out_offset=bass.IndirectOffsetOnAxis(ap=idx[:, ci:ci + 1], axis=0), in_=hb[:], in_offset=None),
                      reads=[hk, "shi_i", "slo_i"], writes=[("Xs", ci * 2 + kk)], dma=f"sc{ci % 2}")

        wg = [C(f"wg{i}", [128, 8, 512], BF16) for i in range(2)]
        wu = [C(f"wu{i}", [128, 8, 512], BF16) for i in range(2)]
        wd = [C(f"wd{i}", [128, 4, D], BF16) for i in range(2)]
        wst = [C(f"wst{i}", [128, 3, 4096], F32) for i in range(2)]
        xs_tok = C("xs_tok", [128, 2, D], BF16)
        xsT = C("xsT", [128, 8, BLK], BF16)
        sgl = C("sgl", [128, BLK], F32)
        aT = C("aT", [128, 4, BLK], BF16)
        ysb = C("ysb", [128, 2, D], F32)
        mmc = [ps(f"mmc{i}", [128, 512], F32, pc_) for i in range(4)]
        tpc = ps("tpc", [128, 8, BLK], BF16, pc_)
        ereg = dd["ereg"]
        S.add("pool", lambda e: e.reg_mov(ereg, NE * 128 - 1))
        nm = [0]

        def nextc():
            p_ = mmc[nm[0] % 4]
            k_ = f"mmc{nm[0] % 4}"
            nm[0] += 1
            return p_, k_

        for b in range(NB):
            bb = b % 2

            st_ = wst[bb]
            for m_, src_d in enumerate((dd["w_gate_d"], dd["w_up_d"], dd["w_down_d"])):
                S.add("pool", lambda e, m_=m_, src_d=src_d, st_=st_, b=b: e.indirect_dma_start(
                    out=st_[:, m_, :], out_offset=None, in_=src_d.rearrange("(r h) f -> r (h f)", h=2),
                    in_offset=bass.IndirectOffsetOnAxis(ap=widx0[:, b:b + 1], axis=0), bounds_check=ereg, oob_is_err=False),
                      reads=["widx"], writes=[(f"wst{bb}", m_)], dma=f"wst{bb}m{m_}")
            S.add("act", lambda e, st_=st_, bb=bb: e.activation(out=wg[bb][:].rearrange("p a b -> p (a b)"), in_=st_[:, 0, :], func=AF.Copy), reads=[(f"wst{bb}", 0)], writes=[f"wg{bb}"])
            S.add("dve", lambda e, st_=st_, bb=bb: e.tensor_copy(out=wu[bb][:].rearrange("p a b -> p (a b)"), in_=st_[:, 1, :]), reads=[(f"wst{bb}", 1)], writes=[f"wu{bb}"])
            S.add("act", lambda e, st_=st_, bb=bb: e.activation(out=wd[bb][:, 0:2, :].rearrange("p a b -> p (a b)"), in_=st_[:, 2, 0:2048], func=AF.Copy), reads=[(f"wst{bb}", 2)], writes=[(f"wd{bb}", 0)])
            S.add("dve", lambda e, st_=st_, bb=bb: e.tensor_copy(out=wd[bb][:, 2:4, :].rearrange("p a b -> p (a b)"), in_=st_[:, 2, 2048:4096]), reads=[(f"wst{bb}", 2)], writes=[(f"wd{bb}", 1)])
            S.add("sp", lambda e, b=b: e.dma_start(out=xs_tok[:], in_=dd["Xs_d"][b * BLK:(b + 1) * BLK, :].rearrange("(r p) d -> p r d", p=128)), reads=["Xs"], writes=["xs_tok"], dma="xsl")
            for r in range(2):
                for k in range(8):
                    S.add("pe", lambda e, r=r, k=k: e.transpose(out=tpc[:, k, r * 128:(r + 1) * 128], in_=xs_tok[:, r, k * 128:(k + 1) * 128], identity=ident), reads=["xs_tok", "cmb"], writes=["tpc"])
            S.add("act", lambda e: e.activation(out=xsT[:].rearrange("p k s -> p (k s)"), in_=tpc[:].rearrange("p k s -> p (k s)"), func=AF.Copy), reads=["tpc"], writes=["xsT"])
            for fc in range(4):
                pg, kg = nextc()
                for k in range(8):
                    S.add("pe", lambda e, k=k, fc=fc, pg=pg, bb=bb: e.matmul(pg[:, 0:BLK], lhsT=wg[bb][:, k, fc * 128:(fc + 1) * 128], rhs=xsT[:, k, :], start=(k == 0), stop=(k == 7)), reads=[f"wg{bb}", "xsT"], writes=[kg])
                pu, ku = nextc()
                for k in range(8):
                    S.add("pe", lambda e, k=k, fc=fc, pu=pu, bb=bb: e.matmul(pu[:, 0:BLK], lhsT=wu[bb][:, k, fc * 128:(fc + 1) * 128], rhs=xsT[:, k, :], start=(k == 0), stop=(k == 7)), reads=[f"wu{bb}", "xsT"], writes=[ku])
                S.add("act", lambda e, pg=pg: e.activation(out=sgl[:], in_=pg[:, 0:BLK], func=AF.Silu), reads=[kg], writes=["sgl"])
                S.add("dve", lambda e, pu=pu, fc=fc: e.tensor_tensor(out=aT[:, fc, :], in0=pu[:, 0:BLK], in1=sgl[:], op=ALU.mult), reads=[ku, "sgl"], writes=[("aT", fc)])
            for half in range(2):
                for dh in range(2):
                    py, ky = nextc()
                    for fc in range(4):
                        S.add("pe", lambda e, fc=fc, half=half, dh=dh, py=py, bb=bb: e.matmul(py[:, 0:512], lhsT=aT[:, fc, half * 128:(half + 1) * 128], rhs=wd[bb][:, fc, dh * 512:(dh + 1) * 512], start=(fc == 0), stop=(fc == 3)),
                              reads=["aT", f"wd{bb}"], writes=[ky])
                    S.add("act", lambda e, half=half, dh=dh, py=py: e.activation(out=ysb[:, half, dh * 512:(dh + 1) * 512], in_=py[:, 0:512], func=AF.Copy), reads=[ky], writes=[("ysb", half * 2 + dh)])
            S.add("sp", lambda e, b=b: e.dma_start(out=dd["Ys_d"][b * BLK:(b + 1) * BLK, :].rearrange("(r p) d -> p r d", p=128), in_=ysb[:]), reads=["ysb"], writes=[("Ys", b)], dma="yst")

        c2.close()
        S.barrier()
        c3 = contextlib.ExitStack()
        C = lambda n, s_, d: sb(n, s_, d, c3)
        ya = [C(f"ya{i}", [128, D], F32) for i in range(2)]
        yb = [C(f"yb{i}", [128, D], F32) for i in range(2)]
        x2c = [C(f"x2c{i}", [128, D], F32) for i in range(2)]
        osb = [C(f"osb{i}", [128, D], F32) for i in range(2)]
        for ci in range(NCHK):
            p = ci % 2
            S.add("pool", lambda e, ci=ci, p=p: e.indirect_dma_start(out=ya[p][:], out_offset=None, in_=dd["Ys_d"], in_offset=bass.IndirectOffsetOnAxis(ap=shi_i[:, ci:ci + 1], axis=0)),
                  reads=["Ys", "shi_i"], writes=[f"ya{p}"], dma=f"ga{p}")
            S.add("pool", lambda e, ci=ci, p=p: e.indirect_dma_start(out=yb[p][:], out_offset=None, in_=dd["Ys_d"], in_offset=bass.IndirectOffsetOnAxis(ap=slo_i[:, ci:ci + 1], axis=0)),
                  reads=["Ys", "slo_i"], writes=[f"yb{p}"], dma=f"gb{p}")
            S.add("sp", lambda e, ci=ci, p=p: e.dma_start(out=x2c[p][:], in_=dd["X2_d"][ci * 128:(ci + 1) * 128, :]), reads=[("X2", ci)], writes=[f"x2c{p}"], dma=f"x2l{p}")
            S.add("dve", lambda e, ci=ci, p=p: e.scalar_tensor_tensor(out=x2c[p][:], in0=ya[p][:], scalar=whi[:, ci:ci + 1], in1=x2c[p][:], op0=ALU.mult, op1=ALU.add), reads=[f"ya{p}", f"x2c{p}", "whi"], writes=[f"x2c{p}"])
            S.add("dve", lambda e, ci=ci, p=p: e.scalar_tensor_tensor(out=x2c[p][:], in0=yb[p][:], scalar=wlo[:, ci:ci + 1], in1=x2c[p][:], op0=ALU.mult, op1=ALU.add), reads=[f"yb{p}", f"x2c{p}", "wlo"], writes=[f"x2c{p}"])
            S.add("act", lambda e, p=p: e.activation(out=osb[p][:], in_=x2c[p][:], func=AF.Square, accum_out=rr[:, p, 0:1]), reads=[f"x2c{p}"], writes=[f"osb{p}", ("rr", p)])
            S.add("act", lambda e, p=p: e.activation(out=rr[:, p, 1:2], in_=rr[:, p, 0:1], func=AF.Ln, scale=1.0 / D, bias=epsr[:, 0:1]), reads=[("rr", p), "eps"], writes=[("rr", p)])
            S.add("act", lambda e, p=p: e.activation(out=rr[:, p, 2:3], in_=rr[:, p, 1:2], func=AF.Exp, scale=-0.5), reads=[("rr", p)], writes=[("rr", p)])
            S.add("dve", lambda e, p=p: e.scalar_tensor_tensor(out=osb[p][:], in0=x2c[p][:], scalar=rr[:, p, 2:3], in1=gfin[:], op0=ALU.mult, op1=ALU.mult), reads=[f"x2c{p}", ("rr", p), "gfin"], writes=[f"osb{p}"])
            S.add("sp", lambda e, ci=ci, p=p: e.dma_start(out=dd["out_d"][ci * 128:(ci + 1) * 128, :], in_=osb[p][:]), reads=[f"osb{p}"], dma=f"ost{p}")
        S.emit_phase(final=True)
        c3.close()
    return None


def make_cm():
    i = np.arange(128)
    ident = np.eye(128, dtype=np.float32)
    ones = np.ones((128, 128), np.float32)
    tri_incl = (i[:, None] <= i[None, :]).astype(np.float32)
    stri_gt = (i[:, None] > i[None, :]).astype(np.float32)
    stri_lt = (i[:, None] < i[None, :]).astype(np.float32)
    return np.ascontiguousarray(np.concatenate([ident, ones, tri_incl, stri_gt, stri_lt], axis=1))


def make_cv(inp, flag):
    cvv = np.zeros((128, NCV), np.float32)

    def put(name, arr):
        o, w = CV[name]
        cvv[:, o:o + w] = arr

    put("g_mix", pk(inp["g_mix"][0], 8))
    cw = np.asarray(inp["conv_w"][0], np.float32)
    put("cw", cw.reshape(31, 8, 128).transpose(2, 1, 0).reshape(128, 8 * 31))
    put("cb", pk(inp["conv_b"][0], 8))
    put("ln_g", pk(inp["ln_g"][0], 8))
    put("ln_b", pk(inp["ln_b"][0], 8))
    w4 = np.asarray(inp["ssd_conv_w"][0], np.float32)
    put("w4", w4.reshape(4, 12, 128).transpose(2, 1, 0).reshape(128, 48))
    put("b4", pk(inp["ssd_conv_b"][0], 12))
    dtb = np.zeros((128, 1), np.float32)
    dtb[0:16, 0] = inp["dt_bias"][0]
    put("dt_bias", dtb)
    put("a_log", np.tile(np.asarray(inp["a_log"][0], np.float32)[None, :], (128, 1)))
    put("dsk", np.tile(np.asarray(inp["d_skip"][0], np.float32)[None, :], (128, 1)))
    put("ng", pk(inp["ssd_norm_g"][0], 8))
    put("g_xa", pk(inp["g_xattn"][0], 8))
    put("g_mem", pk(inp["g_mem"][0], 8))
    put("g_moe", pk(inp["g_moe"][0], 8))
    rb = np.concatenate([np.asarray(inp["b_router_group"][0]), np.asarray(inp["b_router_expert"][0])]).astype(np.float32)
    put("rb", np.tile(rb[None, :], (128, 1)))
    put("flag", np.full((128, 1), flag, np.float32))
    put("thr", np.tile((np.arange(16, dtype=np.float32) * BLK)[None, :], (128, 1)))
    put("iob", np.tile(np.arange(64, dtype=np.float32)[None, :], (128, 1)))
    put("pid2", (2.0 * np.arange(128, dtype=np.float32))[:, None])
    return cvv


def make_in_maps(inp, nt, ncores):
    NTOK = nt * T
    x = np.asarray(inp["x"], np.float32)
    mem = np.asarray(inp["mem"], np.float32)
    cm = make_cm()
    gbc = np.concatenate([np.tile(np.asarray(inp["g_moe"][0], np.float32)[None, :], (128, 1)),
                          np.tile(np.asarray(inp["g_final"], np.float32)[None, :], (128, 1))], axis=1)
    w_r = np.ascontiguousarray(np.concatenate([inp["w_router_group"][0], inp["w_router_expert"][0]], axis=1).astype(np.float32))
    shared = dict(cm=cm, gbc=np.ascontiguousarray(gbc), w_in=np.ascontiguousarray(inp["w_in"][0]), w_out=np.ascontiguousarray(inp["w_out"][0]),
                  w_q=np.ascontiguousarray(inp["w_q"][0]), w_k=np.ascontiguousarray(inp["w_k"][0]), w_v=np.ascontiguousarray(inp["w_v"][0]),
                  w_o=np.ascontiguousarray(inp["w_o"][0]), w_r=w_r, w_gate_r=np.ascontiguousarray(np.asarray(inp["w_gate"][0], np.float32).reshape(NE, 8, 128, 512).transpose(0, 2, 1, 3)).reshape(NE * 256, 2048),
                  w_up_r=np.ascontiguousarray(np.asarray(inp["w_up"][0], np.float32).reshape(NE, 8, 128, 512).transpose(0, 2, 1, 3)).reshape(NE * 256, 2048),
                  w_down_r=np.ascontiguousarray(np.asarray(inp["w_down"][0], np.float32).reshape(NE, 4, 128, D).transpose(0, 2, 1, 3)).reshape(NE * 256, 2048))
    maps = []
    for c in range(ncores):
        b, half = c // 2, c % 2
        xm = x[b, half * NTOK:(half + 1) * NTOK]
        xp = x[b, 0:NTOK] if half == 1 else np.zeros_like(xm)
        xT = np.ascontiguousarray(np.concatenate([xp, xm], axis=0).T)
        m = dict(shared)
        m["xT"] = xT
        m["memT"] = np.ascontiguousarray(mem[b].T)
        m["cv"] = make_cv(inp, float(half))
        maps.append(m)
    return maps


_NC_CACHE = {}


def kernel(**inputs):
    nt = 16
    if nt not in _NC_CACHE:
        _NC_CACHE[nt] = build(nt)
    nc = _NC_CACHE[nt]
    maps = make_in_maps(inputs, nt, 8)
    res = run_bass_kernel_spmd(nc, maps, core_ids=list(range(8)))
    NTOK = nt * T
    out = np.zeros((4, 2 * NTOK, D), np.float32)
    for c in range(8):
        out[c // 2, (c % 2) * NTOK:(c % 2 + 1) * NTOK] = res.results[c]["out"]
    return out
```
